# Optimizing a Trainium2 kernel written in Bass

```python
import jax, jax.numpy as jnp
from jax import lax
import numpy as np

D_MODEL = 1024
BATCH = 32
SEQ = 2048
DEPTH = 2

CHUNK = 64
MIX_WIDTH = D_MODEL
SB_HEAD_DIM = 64
SB_WIDTH = MIX_WIDTH // 2
SB_HEADS = SB_WIDTH // SB_HEAD_DIM
SB_SCALE = SB_HEAD_DIM ** -0.5
Q_BLOCK = 128
SG_GROUP_DIM = 64
SG_WIDTH = MIX_WIDTH - SB_WIDTH
SG_GROUPS = SG_WIDTH // SG_GROUP_DIM
SG_BLOCK = 128
IN_PROJ_WIDTH = 3 * SB_WIDTH + 2 * SG_WIDTH
D_FF = ((8 * D_MODEL // 3 + 127) // 128) * 128
N_EXPERTS = 8
TOP_K = 2
D_EXPERT = 7 * D_MODEL // 2
MOE_BLOCK = 512
N_DENSE = (DEPTH + 1) // 2
N_MOE = DEPTH // 2
EPS = 1e-6

kernel_name = 'hybrid_stickbreak_gmlp_moe_adaln'


def rms_norm(x, g):
    xf = x.astype(jnp.float32)
    y = xf * lax.rsqrt(jnp.mean(xf * xf, axis=-1, keepdims=True) + EPS)
    return (y * g.astype(jnp.float32)).astype(x.dtype)


def layer_norm(x, g, b):
    xf = x.astype(jnp.float32)
    mu = jnp.mean(xf, axis=-1, keepdims=True)
    var = jnp.mean(jnp.square(xf - mu), axis=-1, keepdims=True)
    y = (xf - mu) * lax.rsqrt(var + EPS) * g.astype(jnp.float32) + b.astype(jnp.float32)
    return y.astype(x.dtype)


def stick_breaking_attention(q, k, v):
    S = q.shape[2]
    outs = []
    for i in range(S // Q_BLOCK):
        q0 = i * Q_BLOCK
        kv_len = q0 + Q_BLOCK
        qb = q[:, :, q0:kv_len]
        kb = k[:, :, :kv_len]
        vb = v[:, :, :kv_len]
        z = jnp.einsum('bhqd,bhkd->bhqk', qb, kb).astype(jnp.float32) * SB_SCALE
        q_pos = q0 + jnp.arange(Q_BLOCK)
        k_pos = jnp.arange(kv_len)
        strict = k_pos[None, :] < q_pos[:, None]
        log_keep = jnp.where(strict, jax.nn.log_sigmoid(-z), 0.0)
        log_after = lax.cumsum(log_keep, axis=3, reverse=True) - log_keep
        w = jnp.where(strict, jnp.exp(jax.nn.log_sigmoid(z) + log_after), 0.0)
        outs.append(jnp.einsum('bhqk,bhkd->bhqd', w.astype(vb.dtype), vb))
    return jnp.concatenate(outs, axis=2)


def spatial_gating(u, v, ln_g, ln_b, w_s, b_s):
    B, S, _ = v.shape
    v = layer_norm(v, ln_g, ln_b)
    nb = S // SG_BLOCK
    v = v.reshape(B, nb, SG_BLOCK, SG_GROUPS, SG_GROUP_DIM)
    pos = jnp.arange(SG_BLOCK)
    chunk_causal = (pos[None, :] // CHUNK) <= (pos[:, None] // CHUNK)
    w = jnp.where(chunk_causal[None], w_s, 0.0).astype(v.dtype)
    mixed = jnp.einsum('gij,bnjgc->bnigc', w, v) + b_s.T[None, None, :, :, None].astype(v.dtype)
    return u * mixed.reshape(B, S, SG_WIDTH)


def token_mixer(h, w_in, q_g, k_g, ln_g, ln_b, w_s, b_s, w_out):
    B, S, _ = h.shape
    proj = h @ w_in
    q, k, v, u_sg, v_sg = jnp.split(
        proj, [SB_WIDTH, 2 * SB_WIDTH, 3 * SB_WIDTH, 3 * SB_WIDTH + SG_WIDTH], axis=-1)
    to_heads = lambda t: t.reshape(B, S, SB_HEADS, SB_HEAD_DIM).transpose(0, 2, 1, 3)
    q = rms_norm(to_heads(q), q_g)
    k = rms_norm(to_heads(k), k_g)
    o_a = stick_breaking_attention(q, k, to_heads(v))
    o_a = o_a.transpose(0, 2, 1, 3).reshape(B, S, SB_WIDTH)
    o_b = spatial_gating(jax.nn.gelu(u_sg, approximate=False), jax.nn.gelu(v_sg, approximate=False),
                         ln_g, ln_b, w_s, b_s)
    return jnp.concatenate([o_a, o_b], axis=-1) @ w_out


def swiglu(h, w_gate, w_up, w_down):
    return (jax.nn.silu(h @ w_gate) * (h @ w_up)) @ w_down


def moe_swiglu(h, router_w, router_b, w_gate, w_up, w_down):
    B, S, D = h.shape
    T = B * S
    A = T * TOP_K
    t = h.reshape(T, D)
    logits = (t @ router_w).astype(jnp.float32) + router_b.astype(jnp.float32)
    top_logit, top_e = lax.top_k(logits, TOP_K)
    gate = jax.nn.softmax(top_logit, axis=-1).astype(h.dtype)
    flat_e = top_e.reshape(A)
    flat_tok = jnp.repeat(jnp.arange(T, dtype=jnp.int32), TOP_K)
    flat_gate = gate.reshape(A)
    order = jnp.argsort(flat_e)
    sorted_e = flat_e[order]
    counts = jnp.bincount(flat_e, length=N_EXPERTS)
    seg_start = jnp.cumsum(counts) - counts
    padded = (counts + MOE_BLOCK - 1) // MOE_BLOCK * MOE_BLOCK
    pad_end = jnp.cumsum(padded)
    pad_start = pad_end - padded
    dest = pad_start[sorted_e] + (jnp.arange(A) - seg_start[sorted_e])
    n_blocks = -(-A // MOE_BLOCK) + N_EXPERTS
    P = n_blocks * MOE_BLOCK
    slot_tok = jnp.zeros((P,), jnp.int32).at[dest].set(flat_tok[order])
    slot_gate = jnp.zeros((P,), h.dtype).at[dest].set(flat_gate[order])
    block_e = jnp.minimum(
        jnp.searchsorted(pad_end, jnp.arange(n_blocks) * MOE_BLOCK, side='right'), N_EXPERTS - 1)
    xb = t[slot_tok].reshape(n_blocks, MOE_BLOCK, D)

    def expert_block(args):
        xe, e = args
        return (jax.nn.silu(xe @ w_gate[e]) * (xe @ w_up[e])) @ w_down[e]

    yb = lax.map(expert_block, (xb, block_e)).reshape(P, D)
    y = jax.ops.segment_sum(yb * slot_gate[:, None], slot_tok, num_segments=T)
    return y.reshape(B, S, D)


def setup_inputs(seed: int = 0) -> dict:
    key = jax.random.key(seed)
    ks = jax.random.split(key, 24)
    nrm = lambda k, shape, s: jax.random.normal(k, shape, jnp.float32) * s
    D = D_MODEL
    return {
        'x': nrm(ks[0], (BATCH, SEQ, D), 1.0),
        'c': nrm(ks[1], (BATCH, D), 1.0),
        'ada_w': nrm(ks[2], (DEPTH, D, 6 * D), 0.5 * D ** -0.5),
        'ada_b': nrm(ks[3], (DEPTH, 6 * D), 0.02),
        'norm_mix_g': 1.0 + nrm(ks[4], (DEPTH, D), 0.02),
        'norm_ffn_g': 1.0 + nrm(ks[5], (DEPTH, D), 0.02),
        'w_in': nrm(ks[6], (DEPTH, D, IN_PROJ_WIDTH), D ** -0.5),
        'q_norm_g': 1.0 + nrm(ks[7], (DEPTH, SB_HEAD_DIM), 0.02),
        'k_norm_g': 1.0 + nrm(ks[8], (DEPTH, SB_HEAD_DIM), 0.02),
        'sg_ln_g': 1.0 + nrm(ks[9], (DEPTH, SG_WIDTH), 0.02),
        'sg_ln_b': nrm(ks[10], (DEPTH, SG_WIDTH), 0.02),
        'sg_w_spatial': nrm(ks[11], (DEPTH, SG_GROUPS, SG_BLOCK, SG_BLOCK), 0.5 * SG_BLOCK ** -0.5),
        'sg_b_spatial': 1.0 + nrm(ks[12], (DEPTH, SG_GROUPS, SG_BLOCK), 0.1),
        'w_out': nrm(ks[13], (DEPTH, MIX_WIDTH, D), MIX_WIDTH ** -0.5),
        'ffn_w_gate': nrm(ks[14], (N_DENSE, D, D_FF), D ** -0.5),
        'ffn_w_up': nrm(ks[15], (N_DENSE, D, D_FF), D ** -0.5),
        'ffn_w_down': nrm(ks[16], (N_DENSE, D_FF, D), D_FF ** -0.5),
        'router_w': nrm(ks[17], (N_MOE, D, N_EXPERTS), D ** -0.5),
        'router_b': nrm(ks[18], (N_MOE, N_EXPERTS), 0.01),
        'moe_w_gate': nrm(ks[19], (N_MOE, N_EXPERTS, D, D_EXPERT), D ** -0.5),
        'moe_w_up': nrm(ks[20], (N_MOE, N_EXPERTS, D, D_EXPERT), D ** -0.5),
        'moe_w_down': nrm(ks[21], (N_MOE, N_EXPERTS, D_EXPERT, D), D_EXPERT ** -0.5),
    }


def reference(x, c, ada_w, ada_b, norm_mix_g, norm_ffn_g, w_in, q_norm_g, k_norm_g,
              sg_ln_g, sg_ln_b, sg_w_spatial, sg_b_spatial, w_out,
              ffn_w_gate, ffn_w_up, ffn_w_down, router_w, router_b,
              moe_w_gate, moe_w_up, moe_w_down):
    c_act = jax.nn.silu(c)
    for l in range(DEPTH):
        ada = (c_act @ ada_w[l] + ada_b[l])[:, None, :]
        sh1, sc1, g1, sh2, sc2, g2 = jnp.split(ada, 6, axis=-1)
        h = rms_norm(x, norm_mix_g[l]) * (1.0 + sc1) + sh1
        x = x + g1 * token_mixer(h, w_in[l], q_norm_g[l], k_norm_g[l], sg_ln_g[l], sg_ln_b[l],
                                 sg_w_spatial[l], sg_b_spatial[l], w_out[l])
        h = rms_norm(x, norm_ffn_g[l]) * (1.0 + sc2) + sh2
        if l % 2 == 0:
            j = l // 2
            y = swiglu(h, ffn_w_gate[j], ffn_w_up[j], ffn_w_down[j])
        else:
            j = l // 2
            y = moe_swiglu(h, router_w[j], router_b[j], moe_w_gate[j], moe_w_up[j], moe_w_down[j])
        x = x + g2 * y
    return x
```

```python
import contextlib
import numpy as np
import concourse.bass as bass
import concourse.mybir as mybir
from concourse.bass_utils import run_bass_kernel_spmd

F32 = mybir.dt.float32
BF16 = mybir.dt.bfloat16
AF = mybir.ActivationFunctionType
ALU = mybir.AluOpType
AX = mybir.AxisListType

D = 1024
S = 2048
NCORES = 8
EPS = 1e-6
ENGINES = ("tensor", "scalar", "vector", "gpsimd", "sync")


class Res:
    __slots__ = ("name", "last_w", "readers", "dma_sem", "dma_cnt")

    def __init__(self, name):
        self.name = name
        self.last_w = None
        self.readers = []
        self.dma_sem = None
        self.dma_cnt = 0


class Op:
    __slots__ = ("eng", "fn", "deps", "is_dma", "sem", "val", "signals", "inc")

    def __init__(self, eng, fn, is_dma):
        self.eng = eng
        self.fn = fn
        self.deps = []
        self.is_dma = is_dma
        self.sem = None
        self.val = None
        self.signals = False
        self.inc = 1


class Prog:
    def __init__(self, nc, same_engine_sync=True):
        self.nc = nc
        self.ops = []
        self.stack = contextlib.ExitStack()
        self.same_engine_sync = same_engine_sync
        self.eng_sem = {}
        self.n_sems = 0

    def sbuf(self, name, shape, dtype):
        return self.stack.enter_context(self.nc.sbuf_tensor(name, list(shape), dtype))

    def psum(self, name, shape, dtype):
        return self.stack.enter_context(self.nc.psum_tensor(name, list(shape), dtype))

    def new_sem(self, name):
        self.n_sems += 1
        return self.stack.enter_context(self.nc.semaphore(name))

    def _link(self, op, deps):
        seen = set()
        for d in deps:
            if id(d) in seen:
                continue
            seen.add(id(d))
            if d.eng == op.eng and not d.is_dma:
                if op.eng == "tensor" or not self.same_engine_sync:
                    continue
            op.deps.append(d)
            d.signals = True

    def _add(self, op, reads, writes):
        deps = []
        for r in reads:
            if r.last_w is not None:
                deps.append(r.last_w)
        for w in writes:
            if w.last_w is not None:
                deps.append(w.last_w)
            deps.extend(w.readers)
        self._link(op, deps)
        for r in reads:
            r.readers.append(op)
        for w in writes:
            w.last_w = op
            w.readers = []
        self.ops.append(op)
        return op

    def op(self, eng, fn, reads=(), writes=()):
        return self._add(Op(eng, fn, False), reads, writes)

    def dma(self, eng, out, in_, reads, writes, dest):
        op = Op(eng, lambda e: e.dma_start(out=out, in_=in_), True)
        op.inc = 16
        if dest.dma_sem is None:
            dest.dma_sem = self.new_sem("d_" + dest.name)
        dest.dma_cnt += 16
        op.sem = dest.dma_sem
        op.val = dest.dma_cnt
        op.signals = True
        return self._add(op, reads, writes)

    def fence(self, res_list):
        deps = []
        for r in res_list:
            if r.last_w is not None:
                deps.append(r.last_w)
            deps.extend(r.readers)
        for e in ENGINES:
            op = Op(e, None, False)
            seen = set()
            for d in deps:
                if id(d) in seen:
                    continue
                seen.add(id(d))
                if d.eng == e and not d.is_dma:
                    continue
                op.deps.append(d)
                d.signals = True
            self.ops.append(op)

    def emit(self):
        nc = self.nc
        for e in ENGINES:
            self.eng_sem[e] = self.new_sem("e_" + e)
        cnt = {e: 0 for e in ENGINES}
        for op in self.ops:
            if op.is_dma:
                continue
            if op.signals:
                cnt[op.eng] += 1
                op.sem = self.eng_sem[op.eng]
                op.val = cnt[op.eng]
        streams = {e: [o for o in self.ops if o.eng == e] for e in ENGINES}

        def run(engname):
            def body(eng):
                waited = {}
                for op in streams[engname]:
                    need = {}
                    for d in op.deps:
                        k = id(d.sem)
                        if k not in need or need[k][1] < d.val:
                            need[k] = (d.sem, d.val)
                    for k, (sem, val) in need.items():
                        if waited.get(k, 0) >= val:
                            continue
                        eng.wait_ge(sem, val)
                        waited[k] = val
                    if op.fn is None:
                        continue
                    ins = op.fn(eng)
                    if op.signals:
                        ins.then_inc(op.sem, op.inc)
            return body

        with nc.Block() as block:
            block.tensor(run("tensor"))
            block.scalar(run("scalar"))
            block.vector(run("vector"))
            block.gpsimd(run("gpsimd"))
            block.sync(run("sync"))

    def close(self):
        self.stack.close()


class RR:
    def __init__(self, items):
        self.items = items
        self.i = 0

    def get(self):
        it = self.items[self.i % len(self.items)]
        self.i += 1
        return it


def build(NSEQ=4, dbg=False, same_engine_sync=True, stop_after=None, skip=(), probe=False, zero_mask=True):
    nc = bass.Bass("TRN2", target_bir_lowering=False)

    def din(name, shape):
        return nc.dram_tensor(name, list(shape), F32, kind="ExternalInput").ap()

    x_d = din("x", [NSEQ, S, D])
    cT_d = din("cT", [128, 8, NSEQ])
    ada_w_d = din("ada_w", [2, D, 6 * D])
    ada_b_d = din("ada_b", [2, 128, 48])
    gmix_d = din("gmix", [2, 128, 8])
    gffn_d = din("gffn", [2, 128, 8])
    w_in_d = din("w_in", [2, D, 2560])
    qg_d = din("qg", [2, 128, 1])
    kg_d = din("kg", [2, 128, 1])
    lng_d = din("lng", [2, 128, 512])
    lnb_d = din("lnb", [2, 128, 512])
    wsT_d = din("wsT", [2, 8, 128, 128])
    bsF_d = din("bsF", [2, 128, 4, 512])
    w_out_d = din("w_out", [2, D, D])
    fg_d = din("ffn_g", [D, 2816])
    fu_d = din("ffn_u", [D, 2816])
    fd_d = din("ffn_d", [2816, D])
    rw_d = din("rw", [128, 8, 8])
    rb_d = din("rb", [128, 8])
    mg_d = din("moe_g", [8, D, 3584])
    mu_d = din("moe_u", [8, D, 3584])
    md_d = din("moe_d", [8, 3584, D])
    out_d = nc.dram_tensor("out", [NSEQ, S, D], F32, kind="ExternalOutput").ap()
    dbg_d = None
    if dbg:
        dbg_d = nc.dram_tensor("dbg", [3, S, D], F32, kind="ExternalOutput").ap()

    P = Prog(nc, same_engine_sync=same_engine_sync)

    xT = P.sbuf("xT", [128, 8, S], F32)
    r_x = [[Res(f"x{j}_{c}") for c in range(8)] for j in range(4)]
    hT = P.sbuf("hT", [128, 8, S], BF16)
    r_h = [Res(f"h{j}") for j in range(4)]
    SH = P.sbuf("SH", [128, 8, 4096], BF16)
    r_sh = [Res(f"sh{i}") for i in range(8)]
    ft = P.sbuf("ft", [128, 8, 512], F32)
    r_ft = [Res(f"ft{i}") for i in range(8)]
    bt = P.sbuf("bt", [128, 6, 512], BF16)
    r_bt = [Res(f"bt{i}") for i in range(6)]
    ident = P.sbuf("ident", [128, 128], F32)
    ones_f = P.sbuf("ones_f", [128, 128], F32)
    ones_b = P.sbuf("ones_b", [128, 128], BF16)
    blk_b = P.sbuf("blk_b", [128, 128], BF16)
    negtri = P.sbuf("negtri", [128, 128], BF16)
    neglow = P.sbuf("neglow", [128, 128], BF16)
    maskL = P.sbuf("maskL", [128, 128], F32)
    maskB = P.sbuf("maskB", [128, 128], F32)
    r_const = Res("const")
    cact = P.sbuf("cact", [128, 8, NSEQ], F32)
    adab = P.sbuf("adab", [128, 2, 48], F32)
    adaT = P.sbuf("adaT", [128, 2, 48, NSEQ], F32)
    gmix = P.sbuf("gmixs", [128, 2, 8], F32)
    gffn = P.sbuf("gffns", [128, 2, 8], F32)
    A1 = P.sbuf("A1", [128, 2, 8, NSEQ], F32)
    A2 = P.sbuf("A2", [128, 2, 8, NSEQ], F32)
    r_ada = Res("ada")
    rw = P.sbuf("rws", [128, 8, 8], F32)
    rb = P.sbuf("rbs", [128, 8], F32)
    bsF = P.sbuf("bsFs", [128, 4, 512], F32)
    lng = P.sbuf("lngs", [128, 512], F32)
    lnb = P.sbuf("lnbs", [128, 512], F32)
    wsT = P.sbuf("wsTs", [128, 8, 128], BF16)
    qgs = P.sbuf("qgs", [128, 1], F32)
    kgs = P.sbuf("kgs", [128, 1], F32)
    r_lc = Res("layerconst")
    r_pws_global = [Res("pw0"), Res("pw1")]
    r_ws = Res("wsT")
    sm = P.sbuf("sm", [128, 64], F32)
    r_sm = Res("sm")
    Gt = P.sbuf("Gt", [128, 16, 8], F32)
    r_G = [Res(f"G{j}") for j in range(4)]
    dgt = P.sbuf("dgt", [128, 2, 128], F32)
    r_dg = [Res("dg0"), Res("dg1")]

    pb = [P.psum(f"pb{i}", [128, 512], F32) for i in range(8)]
    r_pb = [Res(f"pb{i}") for i in range(8)]

    r_out = [Res(f"out{i}") for i in range(4)]
    out_ctr = [0]

    def mm(out, lhsT, rhs, start, stop, R, W, sgc=False):
        if sgc:
            P.op("tensor", lambda e: e.matmul(out, lhsT=lhsT, rhs=rhs, start=start, stop=stop, skip_group_check=True), R, W)
        else:
            P.op("tensor", lambda e: e.matmul(out, lhsT=lhsT, rhs=rhs, start=start, stop=stop), R, W)

    def tr(out, in_, R, W):
        P.op("tensor", lambda e: e.transpose(out, in_, ident[:]), list(R) + [r_const], W)

    def act(out, in_, func, R, W, bias=None, scale=None):
        kw = {}
        if bias is not None:
            kw["bias"] = bias
        if scale is not None:
            kw["scale"] = scale
        if func == AF.Copy and not kw:
            P.op("scalar", lambda e: e.copy(out=out, in_=in_), R, W)
        else:
            P.op("scalar", lambda e: e.activation(out=out, in_=in_, func=func, **kw), R, W)

    def vop(eng, name, R, W, **kw):
        P.op(eng, lambda e: getattr(e, name)(**kw), R, W)

    def cols(j):
        return slice(j * 512, (j + 1) * 512)

    dumped = set()

    def dump(name, ap, R):
        if not probe or name in dumped:
            return
        dumped.add(name)
        shape = list(ap.shape)
        d = nc.dram_tensor("p_" + name, shape, F32, kind="ExternalOutput").ap()
        rd = Res("p_" + name)
        P.dma("gpsimd", d, ap, R, [rd], rd)
        r_out.append(rd)

    ftp = RR([(ft[:, i, :], r_ft[i]) for i in range(8)])
    ftp4 = RR([(ft[:, i, :], r_ft[i]) for i in range(4)])
    ftp3 = RR([(ft[:, i, :], r_ft[i]) for i in range(3)])
    btp = RR([(bt[:, i, :], r_bt[i]) for i in range(6)])

    def build_consts():
        g = "gpsimd"
        W = [r_const]
        vop(g, "memset", [], W, ap=ident[:], constant=1.0)
        P.op(g, lambda e: e.affine_select(out=ident[:], in_=ident[:], pattern=[[-1, 128]], compare_op=ALU.is_equal,
                                          fill=0.0, base=0, channel_multiplier=1), W, W)
        vop(g, "memset", [], W, ap=ones_f[:], constant=1.0)
        vop(g, "memset", [], W, ap=ones_b[:], constant=1.0)
        vop(g, "memset", [], W, ap=blk_b[:], constant=0.0)
        vop(g, "memset", W, W, ap=blk_b[0:64, 0:64], constant=1.0 / 64)
        vop(g, "memset", W, W, ap=blk_b[64:128, 64:128], constant=1.0 / 64)
        t0, rt0 = ft[:, 0, 0:128], r_ft[0]
        vop(g, "memset", [], [rt0], ap=t0, constant=-1.0)
        P.op(g, lambda e: e.affine_select(out=t0, in_=t0, pattern=[[-1, 128]], compare_op=ALU.is_gt,
                                          fill=0.0, base=0, channel_multiplier=1), [rt0], [rt0])
        vop(g, "tensor_copy", [rt0], W, out=negtri[:], in_=t0)
        vop(g, "memset", W, [rt0], ap=t0, constant=-1.0)
        P.op(g, lambda e: e.affine_select(out=t0, in_=t0, pattern=[[1, 128]], compare_op=ALU.is_ge,
                                          fill=0.0, base=0, channel_multiplier=-1), [rt0], [rt0])
        vop(g, "tensor_copy", [rt0], W, out=neglow[:], in_=t0)
        vop(g, "memset", [], W, ap=maskL[:], constant=1.0)
        P.op(g, lambda e: e.affine_select(out=maskL[:], in_=maskL[:], pattern=[[1, 128]], compare_op=ALU.is_gt,
                                          fill=0.0, base=0, channel_multiplier=-1), W, W)
        vop(g, "memset", [], W, ap=maskB[:], constant=0.0)
        P.op(g, lambda e: e.affine_select(out=maskB[:], in_=maskB[:], pattern=[[1, 128]], compare_op=ALU.is_gt,
                                          fill=-30000.0, base=0, channel_multiplier=-1), W, W)
        P.dma("sync", cact[:], cT_d, [], [r_ada], r_ada)
        P.dma("sync", adab[:], ada_b_d.rearrange("l p j -> p l j"), [], [r_ada], r_ada)
        P.dma("sync", gmix[:], gmix_d.rearrange("l p j -> p l j"), [], [r_ada], r_ada)
        P.dma("sync", gffn[:], gffn_d.rearrange("l p j -> p l j"), [], [r_ada], r_ada)
        P.dma("sync", rw[:], rw_d, [], [r_const], r_const)
        P.dma("sync", rb[:], rb_d, [], [r_const], r_const)
        act(cact[:], cact[:], AF.Silu, [r_ada], [r_ada])

    def ada_phase():
        stg = [xT[:, 0:2, :].rearrange("p a (k n) -> p (a k) n", n=512),
               xT[:, 2:4, :].rearrange("p a (k n) -> p (a k) n", n=512)]
        r_stg = [Res("adastg0"), Res("adastg1")]
        pa = pb[0]
        for l in range(2):
            for pc in range(12):
                sb = pc % 2
                P.dma("sync", stg[sb], ada_w_d[l, :, pc * 512:(pc + 1) * 512].rearrange("(k p) n -> p k n", p=128),
                      [], [r_stg[sb]], r_stg[sb])
                for cc in range(4):
                    j = pc * 4 + cc
                    for k in range(8):
                        mm(pa[:, j * NSEQ:(j + 1) * NSEQ], stg[sb][:, k, cc * 128:(cc + 1) * 128], cact[:, k, :],
                           k == 0, k == 7, [r_stg[sb], r_ada], [r_pb[0]])
            pav = pa[:, 0:48 * NSEQ].rearrange("p (j b) -> p j b", b=NSEQ)
            for b in range(NSEQ):
                vop("vector", "tensor_tensor", [r_pb[0], r_ada], [r_ada], out=adaT[:, l, :, b], in0=pav[:, :, b],
                    in1=adab[:, l, :], op=ALU.add)
            for b in range(NSEQ):
                vop("vector", "scalar_tensor_tensor", [r_ada], [r_ada], out=A1[:, l, :, b], in0=adaT[:, l, 8:16, b],
                    scalar=1.0, in1=gmix[:, l, :], op0=ALU.add, op1=ALU.mult)
                vop("vector", "scalar_tensor_tensor", [r_ada], [r_ada], out=A2[:, l, :, b], in0=adaT[:, l, 32:40, b],
                    scalar=1.0, in1=gffn[:, l, :], op0=ALU.add, op1=ALU.mult)
        dump('adaT', adaT[:], [r_ada]); dump('A1', A1[:], [r_ada])
        P.fence(r_stg + [r_ada, r_const, r_pb[0]])

    def load_x(s):
        for t in range(16):
            j = t // 4
            xs_i = t % 2
            xs = ft[:, 4 * xs_i:4 * xs_i + 2, :].rearrange("p a n -> p (a n)")
            rxs = [r_ft[4 * xs_i], r_ft[4 * xs_i + 1]]
            P.dma("sync", xs, x_d[s, t * 128:(t + 1) * 128, :], [], rxs, rxs[0])
            bA, bB = (2 * (t % 4)) % 8, (2 * (t % 4) + 1) % 8
            for c in range(8):
                bk = bA if c < 4 else bB
                tr(pb[bk][:, (c % 4) * 128:(c % 4 + 1) * 128], xs[:, c * 128:(c + 1) * 128], rxs, [r_pb[bk]])
            act(xT[:, 0:4, t * 128:(t + 1) * 128], pb[bA][:].rearrange("p (k n) -> p k n", k=4), AF.Copy,
                [r_pb[bA]], r_x[j][0:4])
            vop("vector", "tensor_copy", [r_pb[bB]], r_x[j][4:8], out=xT[:, 4:8, t * 128:(t + 1) * 128],
                in_=pb[bB][:].rearrange("p (k n) -> p k n", k=4))

    def store_x(dst):
        for t in range(16):
            j = t // 4
            xs_i = t % 2
            xs = ft[:, 4 * xs_i:4 * xs_i + 2, :].rearrange("p a n -> p (a n)")
            rxs = [r_ft[4 * xs_i], r_ft[4 * xs_i + 1]]
            bA, bB = (2 * (t % 4)) % 8, (2 * (t % 4) + 1) % 8
            for c in range(8):
                bk = bA if c < 4 else bB
                tr(pb[bk][:, (c % 4) * 128:(c % 4 + 1) * 128], xT[:, c, t * 128:(t + 1) * 128], [r_x[j][c]], [r_pb[bk]])
            act(xs[:, 0:512], pb[bA][:], AF.Copy, [r_pb[bA]], [rxs[0]])
            vop("vector", "tensor_copy", [r_pb[bB]], [rxs[1]], out=xs[:, 512:1024], in_=pb[bB][:])
            ro = r_out[out_ctr[0] % 4]
            out_ctr[0] += 1
            P.dma("sync", dst[t * 128:(t + 1) * 128, :], xs, rxs, [ro], ro)

    def norm_phase(s, l, which):
        A = A1 if which == 1 else A2
        boff = 0 if which == 1 else 24
        router = (l == 1 and which == 2)
        for j in range(4):
            ssum = pb[j % 2]
            r_ss = r_pb[j % 2]
            for c in range(8):
                sq, rsq = btp.get()
                act(sq, xT[:, c, cols(j)], AF.Square, [r_x[j][c]], [rsq])
                mm(ssum[:], ones_b[:], sq, c == 0, c == 7, [rsq, r_const], [r_ss])
            rs, rrs = ft[:, 3, :], r_ft[3]
            act(rs, ssum[:], AF.Ln, [r_ss], [rrs], bias=EPS, scale=1.0 / D)
            act(rs, rs, AF.Exp, [rrs], [rrs], scale=-0.5)
            for c in range(8):
                tmp, rtmp = ftp3.get()
                vop("vector", "scalar_tensor_tensor", [r_x[j][c], rrs, r_ada], [rtmp], out=tmp, in0=xT[:, c, cols(j)],
                    scalar=A[:, l, c, s:s + 1], in1=rs, op0=ALU.mult, op1=ALU.mult)
                bias = adaT[:, l, boff + c, s:s + 1]
                dump('tmp0', tmp, [rtmp])
                act(hT[:, c, cols(j)], tmp, AF.Identity, [rtmp, r_ada], [r_h[j]], bias=bias, scale=1.0)
                if router:
                    hf, rhf = ftp3.get()
                    act(hf, tmp, AF.Identity, [rtmp, r_ada], [rhf], bias=bias, scale=1.0)
                    for sub in range(4):
                        mm(pb[7][:, sub * 8:(sub + 1) * 8], hf[:, sub * 128:(sub + 1) * 128], rw[:, c, :],
                           (c == 0 and sub == 0), c == 7, [rhf, r_const], [r_pb[7]], sgc=True)
            if j == 0 and l == 0 and which == 1:
                dump('h0', hT[:, :, 0:512], [r_h[0]])
            if router:
                for sub in range(4):
                    gating(pb[7][:, sub * 8:(sub + 1) * 8], j * 4 + sub, j)

    def gating(lg_ps, tt, j):
        v = "vector"
        R = [r_sm]
        Lb, m1, mk1, L2, m2, mk2 = sm[:, 0:8], sm[:, 8:9], sm[:, 16:24], sm[:, 24:32], sm[:, 9:10], sm[:, 32:40]
        d, ed, g1, g2, Ga = sm[:, 10:11], sm[:, 11:12], sm[:, 12:13], sm[:, 13:14], sm[:, 40:48]
        vop(v, "tensor_tensor", [r_pb[7], r_const, r_sm], R, out=Lb, in0=lg_ps, in1=rb[:], op=ALU.add)
        vop(v, "tensor_reduce", R, R, out=m1, in_=Lb, axis=AX.X, op=ALU.max)
        vop(v, "tensor_scalar", R, R, out=mk1, in0=Lb, scalar1=m1, scalar2=None, op0=ALU.is_equal)
        vop(v, "scalar_tensor_tensor", R, R, out=L2, in0=mk1, scalar=-1e30, in1=Lb, op0=ALU.mult, op1=ALU.add)
        vop(v, "tensor_reduce", R, R, out=m2, in_=L2, axis=AX.X, op=ALU.max)
        vop(v, "tensor_scalar", R, R, out=mk2, in0=L2, scalar1=m2, scalar2=None, op0=ALU.is_equal)
        vop(v, "tensor_tensor", R, R, out=d, in0=m2, in1=m1, op=ALU.subtract)
        act(ed, d, AF.Exp, R, R)
        vop(v, "tensor_scalar", R, R, out=g1, in0=ed, scalar1=1.0, scalar2=None, op0=ALU.add)
        vop(v, "reciprocal", R, R, out=g1, in_=g1)
        vop(v, "tensor_tensor", R, R, out=g2, in0=ed, in1=g1, op=ALU.mult)
        vop(v, "tensor_scalar", R, R, out=Ga, in0=mk1, scalar1=g1, scalar2=None, op0=ALU.mult)
        vop(v, "scalar_tensor_tensor", R, [r_sm, r_G[j]], out=Gt[:, tt, :], in0=mk2, scalar=g2, in1=Ga,
            op0=ALU.mult, op1=ALU.add)

    def slot_kn(i, w):
        return SH[:, i, 0:8 * w].rearrange("p (k n) -> p k n", k=8)

    def load_cols(i, src2d, c0, w):
        v = slot_kn(i, w)
        P.dma("gpsimd", v, src2d[:, c0:c0 + w].rearrange("(k p) n -> p k n", p=128), [], [r_sh[i]], r_sh[i])
        return v

    def load_rows(i, src2d, r0, nrows):
        nch = nrows // 128
        v = SH[:, i, 0:nch * 1024].rearrange("p (c d) -> p c d", c=nch)
        P.dma("gpsimd", v, src2d[r0:r0 + nrows, :].rearrange("(c p) d -> p c d", p=128), [], [r_sh[i]], r_sh[i])
        return v

    def x_update(s, l, j, dc, ybank, rbank, goff):
        vop("vector", "scalar_tensor_tensor", [rbank, r_ada, r_x[j][dc]], [r_x[j][dc]], out=xT[:, dc, cols(j)],
            in0=ybank[:], scalar=adaT[:, l, goff + dc, s:s + 1], in1=xT[:, dc, cols(j)], op0=ALU.mult, op1=ALU.add)

    def mixer_phase(s, l):
        W = [r_lc]
        P.dma("sync", bsF[:], bsF_d[l], [], W, r_lc)
        P.dma("sync", lng[:], lng_d[l], [], W, r_lc)
        P.dma("sync", lnb[:], lnb_d[l], [], W, r_lc)
        P.dma("sync", qgs[:], qg_d[l], [], W, r_lc)
        P.dma("sync", kgs[:], kg_d[l], [], W, r_lc)
        P.dma("gpsimd", wsT[:], wsT_d[l].rearrange("g j i -> j g i"), [], [r_ws], r_ws)
        vop("gpsimd", "memset", [r_ws], [r_ws], ap=wsT[64:128, :, 0:64], constant=0.0)
        vop("vector", "tensor_scalar", W, W, out=qgs[:], in0=qgs[:], scalar1=0.125, scalar2=None, op0=ALU.mult)

        Wvg = load_cols(1, w_in_d[l], 2048, 512)
        Wu = load_cols(2, w_in_d[l], 1536, 512)
        WoB = load_rows(3, w_out_d[l], 512, 512)
        vn = SH[:, 5, 2048:4096].rearrange("p (b n) -> p b n", b=4)
        r_vn = r_sh[5]
        obs = [SH[:, 7, 0:2048].rearrange("p (c n) -> p c n", c=4), SH[:, 7, 2048:4096].rearrange("p (c n) -> p c n", c=4)]
        r_ob = [Res("ob0"), Res("ob1")]
        r_spb = [Res(f"spb{i}") for i in range(8)]
        qTs = [SH[:, 4, 0:2048], SH[:, 2, 0:2048]]
        kTs = [SH[:, 4, 2048:4096], SH[:, 2, 2048:4096]]
        vts = [SH[:, 5, 0:2048].rearrange("p (t n) -> p t n", t=16), SH[:, 5, 2048:4096].rearrange("p (t n) -> p t n", t=16)]
        pws = [SH[:, 0, 0:3072].rearrange("p (i k n) -> p i k n", i=3, k=8), SH[:, 3, 0:3072].rearrange("p (i k n) -> p i k n", i=3, k=8)]
        WoPs = [SH[:, 0, 3072:4096], SH[:, 3, 3072:4096]]
        r_qs, r_ks, r_vts = ([Res(f"{n}{i}") for i in range(2)] for n in ("q", "k", "vt"))
        r_pws = r_pws_global
        oas = [SH[:, 6, 0:2048], SH[:, 6, 2048:4096]]
        r_oa = [[Res(f"oa{i}_{g}") for g in range(4)] for i in range(2)]
        PB = 6

        def proj_gen(cc):
            b = cc % 2
            pw, r_pw = pws[b], r_pws[b]
            qf, rqf = ft[:, 6, :], r_ft[6]
            rs, rrs = ft[:, 7, :], r_ft[7]
            sq, rsq = bt[:, 5, :], r_bt[5]
            for i, c0 in enumerate((cc * 128, 512 + cc * 128, 1024 + cc * 128)):
                P.dma("gpsimd", pw[:, i, :, :], w_in_d[l][:, c0:c0 + 128].rearrange("(k p) n -> p k n", p=128),
                      [], [r_pw], r_pw)
            P.dma("gpsimd", WoPs[b], w_out_d[l][cc * 128:(cc + 1) * 128, :], [], [r_pw], r_pw)
            yield
            for j in range(4):
                for i, (dst, rdst, gvec) in enumerate(((qTs[b], r_qs[b], qgs), (kTs[b], r_ks[b], kgs))):
                    for k in range(8):
                        mm(pb[PB][:], pw[:, i, k, :], hT[:, k, cols(j)], k == 0, k == 7, [r_h[j], r_pw], [r_pb[PB]])
                        if k % 2 == 1:
                            yield
                    act(qf, pb[PB][:], AF.Copy, [r_pb[PB]], [rqf])
                    act(sq, pb[PB][:], AF.Square, [r_pb[PB]], [rsq])
                    yield
                    mm(pb[PB][:], blk_b[:], sq, True, True, [rsq, r_const], [r_pb[PB]])
                    yield
                    act(rs, pb[PB][:], AF.Ln, [r_pb[PB]], [rrs], bias=EPS, scale=1.0)
                    act(rs, rs, AF.Exp, [rrs], [rrs], scale=-0.5)
                    yield
                    vop("vector", "scalar_tensor_tensor", [rqf, rrs, r_lc], [rdst], out=dst[:, cols(j)], in0=qf,
                        scalar=gvec[:, 0:1], in1=rs, op0=ALU.mult, op1=ALU.mult)
                    yield
                for blk in range(4):
                    tcols = slice(j * 512 + blk * 128, j * 512 + (blk + 1) * 128)
                    for k in range(8):
                        mm(pb[PB][:, blk * 128:(blk + 1) * 128], hT[:, k, tcols], pw[:, 2, k, :], k == 0, k == 7,
                           [r_h[j], r_pw], [r_pb[PB]])
                        if k % 4 == 3:
                            yield
                act(vts[b][:, j * 4:(j + 1) * 4, :], pb[PB][:].rearrange("p (t n) -> p t n", t=4), AF.Copy,
                    [r_pb[PB]], [r_vts[b]])
                yield

        r_gst = [Res(f"gst{i}") for i in range(4)]
        mvv = sm[:, 24:32].rearrange("p (b t) -> p b t", t=2)
        ftp_hi = RR([(ft[:, 4 + i, :], r_ft[4 + i]) for i in range(2)])
        def gate_gen():
            for j in (range(4) if 'gate' not in skip else ()):
                for blk in range(4):
                    tcols = slice(j * 512 + blk * 128, j * 512 + (blk + 1) * 128)
                    bk = blk
                    for k in range(8):
                        mm(pb[bk][:], hT[:, k, tcols], Wvg[:, k, :], k == 0, k == 7, [r_h[j], r_sh[1]], [r_pb[bk]])
                    gv, rgv = ft[:, blk, :], r_ft[blk]
                    act(gv, pb[bk][:], AF.Gelu, [r_pb[bk]], [rgv])
                    vop("vector", "bn_stats", [rgv], [r_gst[blk]], out=sm[:, 6 * blk:6 * blk + 6], in_=gv)
                    vop("vector", "bn_aggr", [r_gst[blk], r_sm], [r_gst[blk]], out=mvv[:, blk, :], in_=sm[:, 6 * blk:6 * blk + 6])
                    yield
                act(sm[:, 32:36], mvv[:, :, 1], AF.Sqrt, r_gst + [r_sm], [r_sm], bias=EPS, scale=1.0)
                vop("vector", "reciprocal", [r_sm], [r_sm], out=sm[:, 32:36], in_=sm[:, 32:36])
                for blk in range(4):
                    gv, rgv = ft[:, blk, :], r_ft[blk]
                    vop("vector", "tensor_scalar", [rgv, r_sm, r_gst[blk]], [rgv], out=gv, in0=gv, scalar1=mvv[:, blk, 0:1],
                        scalar2=sm[:, 32 + blk:33 + blk], op0=ALU.subtract, op1=ALU.mult)
                    vop("vector", "tensor_tensor", [rgv, r_lc], [rgv], out=gv, in0=gv, in1=lng[:], op=ALU.mult)
                    vop("gpsimd", "tensor_tensor", [rgv, r_lc], [r_vn], out=vn[:, blk, :], in0=gv, in1=lnb[:], op=ALU.add)
                    yield
                ob = obs[j % 2]
                rob = r_ob[j % 2]
                dump('vn0', vn, [r_vn])
                for cc in range(4):
                    for blk in range(4):
                        for gi in range(2):
                            bk = 2 + gi
                            mm(pb[bk][:, blk * 128:(blk + 1) * 128], vn[:, blk, cc * 128:(cc + 1) * 128],
                               wsT[:, 2 * cc + gi, :], True, True, [r_vn, r_ws], [r_pb[bk]])
                    mT, rmT = ftp_hi.get()
                    vop("vector", "tensor_tensor", [r_pb[2], r_lc], [rmT], out=mT[0:64, :], in0=pb[2][0:64, :],
                        in1=bsF[0:64, cc, :], op=ALU.add)
                    vop("vector", "tensor_tensor", [r_pb[3], r_lc], [rmT], out=mT[64:128, :], in0=pb[3][64:128, :],
                        in1=bsF[64:128, cc, :], op=ALU.add)
                    yield
                    bk = 4 + cc % 2
                    for k in range(8):
                        mm(pb[bk][:], Wu[:, k, cc * 128:(cc + 1) * 128], hT[:, k, cols(j)], k == 0, k == 7,
                           [r_h[j], r_sh[2]], [r_pb[bk]])
                    gu, rgu = ftp_hi.get()
                    act(gu, pb[bk][:], AF.Gelu, [r_pb[bk]], [rgu])
                    vop("gpsimd", "tensor_tensor", [rgu, rmT], [rob], out=ob[:, cc, :], in0=gu, in1=mT, op=ALU.mult)
                    yield
                dump('ob0', ob, [rob])
                for dc in range(8):
                    bk = (7, 0)[dc % 2]
                    for k4 in range(4):
                        mm(pb[bk][:], WoB[:, k4, dc * 128:(dc + 1) * 128], ob[:, k4, :], k4 == 0, k4 == 3,
                           [rob, r_sh[3]], [r_pb[bk]])
                    x_update(s, l, j, dc, pb[bk], r_pb[bk], 16)
                    if dc % 2 == 1:
                        yield


        pairs = list(range(4)) if 'attn' not in skip else []
        gens = []
        if 'gate' not in skip:
            gens.append(gate_gen())
        if pairs:
            gens.append(proj_gen(0))
        while gens:
            for g_ in list(gens):
                try:
                    next(g_)
                except StopIteration:
                    gens.remove(g_)
        P.fence([r_sh[1], r_sh[2], r_sh[3], r_sh[5], r_sh[7]] + r_ob)
        for cc in pairs:
            b = cc % 2
            qT, kT, vt, WoP = qTs[b], kTs[b], vts[b], WoPs[b]
            r_q, r_k, r_vt, r_pw = r_qs[b], r_ks[b], r_vts[b], r_pws[b]
            nxt = proj_gen(cc + 1) if cc + 1 < 4 else iter(())
            oa = oas[cc % 2]
            roa = r_oa[cc % 2]
            steps = []
            for g in range(4):
                for kb in range(4 * g + 3, -1, -1):
                    for h in range(2):
                        col0 = max(0, kb * 128 - g * 512)
                        steps.append(dict(g=g, kb=kb, h=h, po=64 * h, first=(kb == 4 * g + 3), last=(kb == 0),
                                          diag=(kb >= 4 * g), col0=col0, wd=512 - col0))
            e_pool = RR([(ft[:, i, :], r_ft[i]) for i in range(6)])
            spb_pool = RR([(SH[:, 1, i * 512:(i + 1) * 512], r_spb[i]) for i in range(8)])
            wt_pool = RR([(bt[:, i, :], r_bt[i]) for i in range(5)])
            sbanks = [0, 1, 7]

            def stS(st, t):
                po, wd, kb, g, col0 = st["po"], st["wd"], st["kb"], st["g"], st["col0"]
                bk = sbanks[t % 3]
                st["S"], st["rS"] = pb[bk], r_pb[bk]
                mm(pb[bk][:, 0:wd], kT[po:po + 64, kb * 128:(kb + 1) * 128],
                   qT[po:po + 64, g * 512 + col0:(g + 1) * 512], True, True, [r_q, r_k], [r_pb[bk]])

            def stA1(st):
                wd = st["wd"]
                e_, re_ = e_pool.get()
                spb, rspb = spb_pool.get()
                st["e"], st["re"], st["spb"], st["rspb"] = e_, re_, spb, rspb
                act(e_[:, 0:wd], st["S"][:, 0:wd], AF.Exp, [st["rS"]], [re_])
                act(spb[:, 0:wd], e_[:, 0:wd], AF.Ln, [re_], [rspb], bias=1.0, scale=1.0)
                if st["diag"]:
                    vop("gpsimd", "tensor_tensor", [rspb, r_const], [rspb], out=spb[:, 0:128], in0=spb[:, 0:128],
                        in1=maskL[:], op=ALU.mult)

            def stA2(st):
                wd = st["wd"]
                vop("vector", "tensor_tensor", [st["rS"], st["rspb"], st["re"]], [st["re"]], out=st["e"][:, 0:wd],
                    in0=st["S"][:, 0:wd], in1=st["spb"][:, 0:wd], op=ALU.subtract)

            def stB1(st):
                h, wd, col0 = st["h"], st["wd"], st["col0"]
                mm(pb[2 + h][:, col0:512], negtri[:], st["spb"][:, 0:wd], st["first"], True,
                   [st["rspb"], r_const], [r_pb[2 + h]], sgc=True)

            def stB2(st):
                h, wd, col0 = st["h"], st["wd"], st["col0"]
                e_, re_ = st["e"], st["re"]
                vop("vector", "tensor_tensor", [r_pb[2 + h], re_], [re_], out=e_[:, 0:wd], in0=pb[2 + h][:, col0:512],
                    in1=e_[:, 0:wd], op=ALU.add)
                if st["diag"]:
                    vop("vector", "tensor_tensor", [re_, r_const], [re_], out=e_[:, 0:128], in0=e_[:, 0:128],
                        in1=maskB[:], op=ALU.add)

            def stB3(st):
                wd = st["wd"]
                wt, rwt = wt_pool.get()
                st["wt"], st["rwt"] = wt, rwt
                act(wt[:, 0:wd], st["e"][:, 0:wd], AF.Exp, [st["re"]], [rwt])

            def stC1(st):
                h, wd, col0 = st["h"], st["wd"], st["col0"]
                mm(pb[2 + h][:, col0:512], neglow[:], st["spb"][:, 0:wd], False, True,
                   [st["rspb"], r_const], [r_pb[2 + h]], sgc=True)

            def stC2(st):
                h, wd, col0, kb, g, po = st["h"], st["wd"], st["col0"], st["kb"], st["g"], st["po"]
                Ob, rO = pb[4 + h], r_pb[4 + h]
                mm(Ob[:, col0:512], vt[:, kb, :], st["wt"][:, 0:wd], st["first"], st["last"], [r_vt, st["rwt"]], [rO],
                   sgc=True)
                if st["last"]:
                    act(oa[po:po + 64, cols(g)], Ob[po:po + 64, :], AF.Copy, [rO], [roa[g]])
                    if h == 1:
                        j = g
                        for dc in range(8):
                            wb = 4 + dc % 2
                            mm(pb[wb][:], WoP[:, dc * 128:(dc + 1) * 128], oa[:, cols(j)], True, True,
                               [roa[j], r_pw], [r_pb[wb]])
                            x_update(s, l, j, dc, pb[wb], r_pb[wb], 16)

            T = len(steps)

            def at(i):
                return steps[i] if 0 <= i < T else None

            for t in range(-1, T + 6):
                for fn, i in ((stC1, t - 4), (stB1, t - 2), (stC2, t - 5)):
                    if at(i) is not None:
                        fn(at(i))
                if at(t + 1) is not None:
                    stS(at(t + 1), t + 1)
                for fn, i in ((stB3, t - 4), (stA1, t), (stB2, t - 3), (stA2, t - 1)):
                    if at(i) is not None:
                        fn(at(i))
                if t >= 2:
                    next(nxt, None)
                    if t % 3 == 0:
                        next(nxt, None)
            for _ in nxt:
                pass
        P.fence(r_sh + r_ob + r_spb + r_qs + r_ks + r_vts + r_pws + r_oa[0] + r_oa[1] + r_h + r_pb + r_ft + r_bt)

    def ffn_phase(s, l):
        if l == 0:
            groups = [(None, gd, fg_d, fu_d, fd_d, f0, fw) for gd, (f0, fw) in
                      enumerate([(0, 512), (512, 512), (1024, 512), (1536, 512), (2048, 512), (2560, 256)])]
        else:
            groups = [(e, fgi, mg_d[e], mu_d[e], md_d[e], fgi * 512, 512) for e in range(8) for fgi in range(7)]
        hids = [SH[:, 6, 0:2048].rearrange("p (c n) -> p c n", c=4), SH[:, 6, 2048:4096].rearrange("p (c n) -> p c n", c=4)]
        r_hid = [Res("hid0"), Res("hid1")]
        hm = SH[:, 7, :].rearrange("p (k n) -> p k n", k=8)
        r_hmk = [Res(f"hm{k}") for k in range(8)]
        gbc = [ft[:, 4 + j, :] for j in range(4)]
        r_gbc = [r_ft[4 + j] for j in range(4)]
        slot_ctr = [0]

        def issue_loads(grp):
            e, gi, gsrc, usrc, dsrc, f0, fw = grp
            base = (slot_ctr[0] % 2) * 3
            slot_ctr[0] += 1
            Wg = load_cols(base, gsrc, f0, fw)
            Wu = load_cols(base + 1, usrc, f0, fw)
            Wd = load_rows(base + 2, dsrc, f0, fw)
            return (Wg, Wu, Wd, base)

        pend = issue_loads(groups[0])
        hctr = 0
        yctr = 0
        for gidx, grp in enumerate(groups):
            e, gi, gsrc, usrc, dsrc, f0, fw = grp
            Wg, Wu, Wd, base = pend
            if gidx + 1 < len(groups):
                pend = issue_loads(groups[gidx + 1])
            nfc = fw // 128
            if e is not None and gi == 0:
                for tt in range(16):
                    j = tt // 4
                    dg, rdg = dgt[:, tt % 2, :], r_dg[tt % 2]
                    vop("vector", "tensor_scalar", [r_G[j], r_const], [rdg], out=dg, in0=ident[:],
                        scalar1=Gt[:, tt, e:e + 1], scalar2=None, op0=ALU.mult)
                    bk = 6 + j % 2
                    mm(pb[bk][:, (tt % 4) * 128:(tt % 4 + 1) * 128], ones_f[:], dg, True, True, [rdg, r_const], [r_pb[bk]])
                    if tt % 4 == 3:
                        act(gbc[j], pb[bk][:], AF.Copy, [r_pb[bk]], [r_gbc[j]])
                        if zero_mask:
                            vop("vector", "tensor_single_scalar", [r_gbc[j]], [r_bt[j]], out=bt[:, j, :], in_=gbc[j],
                                scalar=0.0, op=ALU.is_gt)
            use_mask = e is not None and zero_mask

            def mask_one(jj, k):
                vop("vector" if k % 2 == 0 else "gpsimd", "tensor_tensor", [r_bt[jj], r_h[jj]], [r_hmk[k]],
                    out=hm[:, k, :], in0=hT[:, k, cols(jj)], in1=bt[:, jj, :], op=ALU.mult)

            if use_mask and gi == 0:
                for k in range(8):
                    mask_one(0, k)
            nxt_same_expert = gidx + 1 < len(groups) and groups[gidx + 1][0] == e
            for j in range(4):
                hid = hids[hctr % 2]
                rhid = r_hid[hctr % 2]
                hctr += 1
                if use_mask:
                    hsrc = lambda k, j=j: hm[:, k, :]
                    rh = r_hmk
                else:
                    hsrc = lambda k, j=j: hT[:, k, cols(j)]
                    rh = [r_h[j]]
                for fc in range(nfc):
                    bg, bu = fc % 2, 2 + fc % 2
                    for k in range(8):
                        mm(pb[bg][:], Wg[:, k, fc * 128:(fc + 1) * 128], hsrc(k), k == 0, k == 7,
                           rh + [r_sh[base]], [r_pb[bg]])
                    for k in range(8):
                        mm(pb[bu][:], Wu[:, k, fc * 128:(fc + 1) * 128], hsrc(k), k == 0, k == 7,
                           rh + [r_sh[base + 1]], [r_pb[bu]])
                    sg, rsg = ftp4.get()
                    act(sg, pb[bg][:], AF.Silu, [r_pb[bg]], [rsg])
                    if use_mask:
                        vop("gpsimd", "tensor_tensor", [rsg, r_gbc[j]], [rsg], out=sg, in0=sg, in1=gbc[j], op=ALU.mult)
                    vop("vector", "tensor_tensor", [rsg, r_pb[bu]], [rhid], out=hid[:, fc, :], in0=pb[bu][:], in1=sg,
                        op=ALU.mult)
                if use_mask and j < 3:
                    nj = j + 1
                elif use_mask and nxt_same_expert:
                    nj = 0
                else:
                    nj = None
                for dc in range(8):
                    bk = 4 + yctr % 4
                    yctr += 1
                    for fc in range(nfc):
                        mm(pb[bk][:], Wd[:, fc, dc * 128:(dc + 1) * 128], hid[:, fc, :], fc == 0, fc == nfc - 1,
                           [rhid, r_sh[base + 2]], [r_pb[bk]])
                    if e is None or use_mask:
                        x_update(s, l, j, dc, pb[bk], r_pb[bk], 40)
                    else:
                        tmp, rtmp = ftp4.get()
                        vop("vector", "scalar_tensor_tensor", [r_pb[bk], r_ada, r_gbc[j]], [rtmp], out=tmp, in0=pb[bk][:],
                            scalar=adaT[:, l, 40 + dc, s:s + 1], in1=gbc[j], op0=ALU.mult, op1=ALU.mult)
                        vop("gpsimd", "tensor_tensor", [rtmp, r_x[j][dc]], [r_x[j][dc]], out=xT[:, dc, cols(j)],
                            in0=xT[:, dc, cols(j)], in1=tmp, op=ALU.add)
                    if nj is not None:
                        mask_one(nj, dc)
        P.fence(r_sh + r_hid + r_h + r_pb + r_ft + r_hmk)

    build_consts()
    ada_phase()
    stage = [0]

    def dbg_dump(s):
        if dbg and s == 0 and stage[0] < 3:
            store_x(dbg_d[stage[0]])
            P.fence(r_ft + r_pb)
        stage[0] += 1

    for s in range(NSEQ):
        stage[0] = 0
        load_x(s)
        P.fence(r_ft + r_pb)
        done = False
        for l in range(2):
            norm_phase(s, l, 1)
            mixer_phase(s, l)
            dbg_dump(s)
            if stop_after == (l, "mix"):
                done = True
                break
            norm_phase(s, l, 2)
            ffn_phase(s, l)
            if l == 0:
                dbg_dump(s)
            if stop_after == (l, "ffn"):
                done = True
                break
        store_x(out_d[s])
        P.fence(r_ft + r_pb + [r_x[j][c] for j in range(4) for c in range(8)])
    P.op("sync", None, r_out, [])
    P.emit()
    P.close()
    return nc, len(P.ops)


def make_in_map(inp, b0, nseq):
    f = lambda a: np.ascontiguousarray(np.asarray(a, dtype=np.float32))
    fm = lambda v, n: f(np.asarray(v).reshape(2, n, 128).transpose(0, 2, 1))
    c = np.asarray(inp["c"])[b0:b0 + nseq]
    bs = np.asarray(inp["sg_b_spatial"])
    bsF = np.repeat(bs, 64, axis=1).reshape(2, 4, 128, 128)
    bsF = np.tile(bsF, (1, 1, 1, 4)).transpose(0, 2, 1, 3)
    m = {
        "x": f(np.asarray(inp["x"])[b0:b0 + nseq]),
        "cT": f(c.T.reshape(8, 128, nseq).transpose(1, 0, 2)),
        "ada_w": f(inp["ada_w"]),
        "ada_b": fm(inp["ada_b"], 48),
        "gmix": fm(inp["norm_mix_g"], 8),
        "gffn": fm(inp["norm_ffn_g"], 8),
        "w_in": f(inp["w_in"]),
        "qg": f(np.tile(np.asarray(inp["q_norm_g"]), (1, 2)).reshape(2, 128, 1)),
        "kg": f(np.tile(np.asarray(inp["k_norm_g"]), (1, 2)).reshape(2, 128, 1)),
        "lng": f(np.broadcast_to(np.asarray(inp["sg_ln_g"])[:, None, :], (2, 128, 512))),
        "lnb": f(np.broadcast_to(np.asarray(inp["sg_ln_b"])[:, None, :], (2, 128, 512))),
        "wsT": f(np.asarray(inp["sg_w_spatial"]).transpose(0, 1, 3, 2)),
        "bsF": f(bsF),
        "w_out": f(inp["w_out"]),
        "ffn_g": f(np.asarray(inp["ffn_w_gate"])[0]),
        "ffn_u": f(np.asarray(inp["ffn_w_up"])[0]),
        "ffn_d": f(np.asarray(inp["ffn_w_down"])[0]),
        "rw": f(np.asarray(inp["router_w"])[0].reshape(8, 128, 8).transpose(1, 0, 2)),
        "rb": f(np.broadcast_to(np.asarray(inp["router_b"])[0][None, :], (128, 8))),
        "moe_g": f(np.asarray(inp["moe_w_gate"])[0]),
        "moe_u": f(np.asarray(inp["moe_w_up"])[0]),
        "moe_d": f(np.asarray(inp["moe_w_down"])[0]),
    }
    return m


_NC_CACHE = {}


def kernel(**inputs):
    nseq = 4
    if "nc" not in _NC_CACHE:
        _NC_CACHE["nc"] = build(NSEQ=nseq)[0]
    nc = _NC_CACHE["nc"]
    shared = make_in_map(inputs, 0, nseq)
    in_maps = []
    for core in range(NCORES):
        m = dict(shared)
        c = np.asarray(inputs["c"])[core * nseq:(core + 1) * nseq]
        m["x"] = np.ascontiguousarray(np.asarray(inputs["x"], dtype=np.float32)[core * nseq:(core + 1) * nseq])
        m["cT"] = np.ascontiguousarray(c.T.reshape(8, 128, nseq).transpose(1, 0, 2).astype(np.float32))
        in_maps.append(m)
    res = run_bass_kernel_spmd(nc, in_maps, core_ids=list(range(NCORES)))
    out = np.concatenate([np.asarray(r["out"]) for r in res.results], axis=0)
    return out.astype(np.float32)
```

```python
import contextlib
import numpy as np
import concourse.bass as bass
import concourse.mybir as mybir
from concourse.bass_utils import run_bass_kernel_spmd

F32 = mybir.dt.float32
BF16 = mybir.dt.bfloat16
AF = mybir.ActivationFunctionType
ALU = mybir.AluOpType
AX = mybir.AxisListType

D = 1024
S = 2048
NCORES = 8
EPS = 1e-6
ENGINES = ("tensor", "scalar", "vector", "gpsimd", "sync")


class Res:
    __slots__ = ("name", "last_w", "readers", "dma_sem", "dma_cnt")

    def __init__(self, name):
        self.name = name
        self.last_w = None
        self.readers = []
        self.dma_sem = None
        self.dma_cnt = 0


class Op:
    __slots__ = ("eng", "fn", "deps", "is_dma", "sem", "val", "signals", "inc")

    def __init__(self, eng, fn, is_dma):
        self.eng = eng
        self.fn = fn
        self.deps = []
        self.is_dma = is_dma
        self.sem = None
        self.val = None
        self.signals = False
        self.inc = 1


class Prog:
    def __init__(self, nc, same_engine_sync=True):
        self.nc = nc
        self.ops = []
        self.stack = contextlib.ExitStack()
        self.same_engine_sync = same_engine_sync
        self.eng_sem = {}
        self.n_sems = 0

    def sbuf(self, name, shape, dtype):
        return self.stack.enter_context(self.nc.sbuf_tensor(name, list(shape), dtype))

    def psum(self, name, shape, dtype):
        return self.stack.enter_context(self.nc.psum_tensor(name, list(shape), dtype))

    def new_sem(self, name):
        self.n_sems += 1
        return self.stack.enter_context(self.nc.semaphore(name))

    def _link(self, op, deps):
        seen = set()
        for d in deps:
            if id(d) in seen:
                continue
            seen.add(id(d))
            if d.eng == op.eng and not d.is_dma:
                if op.eng == "tensor" or not self.same_engine_sync:
                    continue
            op.deps.append(d)
            d.signals = True

    def _add(self, op, reads, writes):
        deps = []
        for r in reads:
            if r.last_w is not None:
                deps.append(r.last_w)
        for w in writes:
            if w.last_w is not None:
                deps.append(w.last_w)
            deps.extend(w.readers)
        self._link(op, deps)
        for r in reads:
            r.readers.append(op)
        for w in writes:
            w.last_w = op
            w.readers = []
        self.ops.append(op)
        return op

    def op(self, eng, fn, reads=(), writes=()):
        return self._add(Op(eng, fn, False), reads, writes)

    def dma(self, eng, out, in_, reads, writes, dest):
        op = Op(eng, lambda e: e.dma_start(out=out, in_=in_), True)
        op.inc = 16
        if dest.dma_sem is None:
            dest.dma_sem = self.new_sem("d_" + dest.name)
        dest.dma_cnt += 16
        op.sem = dest.dma_sem
        op.val = dest.dma_cnt
        op.signals = True
        return self._add(op, reads, writes)

    def fence(self, res_list):
        deps = []
        for r in res_list:
            if r.last_w is not None:
                deps.append(r.last_w)
            deps.extend(r.readers)
        for e in ENGINES:
            op = Op(e, None, False)
            seen = set()
            for d in deps:
                if id(d) in seen:
                    continue
                seen.add(id(d))
                if d.eng == e and not d.is_dma:
                    continue
                op.deps.append(d)
                d.signals = True
            self.ops.append(op)

    def emit(self):
        nc = self.nc
        for e in ENGINES:
            self.eng_sem[e] = self.new_sem("e_" + e)
        cnt = {e: 0 for e in ENGINES}
        for op in self.ops:
            if op.is_dma:
                continue
            if op.signals:
                cnt[op.eng] += 1
                op.sem = self.eng_sem[op.eng]
                op.val = cnt[op.eng]
        streams = {e: [o for o in self.ops if o.eng == e] for e in ENGINES}

        def run(engname):
            def body(eng):
                waited = {}
                for op in streams[engname]:
                    need = {}
                    for d in op.deps:
                        k = id(d.sem)
                        if k not in need or need[k][1] < d.val:
                            need[k] = (d.sem, d.val)
                    for k, (sem, val) in need.items():
                        if waited.get(k, 0) >= val:
                            continue
                        eng.wait_ge(sem, val)
                        waited[k] = val
                    if op.fn is None:
                        continue
                    ins = op.fn(eng)
                    if op.signals:
                        ins.then_inc(op.sem, op.inc)
            return body

        with nc.Block() as block:
            block.tensor(run("tensor"))
            block.scalar(run("scalar"))
            block.vector(run("vector"))
            block.gpsimd(run("gpsimd"))
            block.sync(run("sync"))

    def close(self):
        self.stack.close()


class RR:
    def __init__(self, items):
        self.items = items
        self.i = 0

    def get(self):
        it = self.items[self.i % len(self.items)]
        self.i += 1
        return it


def build(NSEQ=4, dbg=False, same_engine_sync=True, stop_after=None, skip=(), probe=False):
    nc = bass.Bass("TRN2", target_bir_lowering=False)

    def din(name, shape):
        return nc.dram_tensor(name, list(shape), F32, kind="ExternalInput").ap()

    x_d = din("x", [NSEQ, S, D])
    cT_d = din("cT", [128, 8, NSEQ])
    ada_w_d = din("ada_w", [2, D, 6 * D])
    ada_b_d = din("ada_b", [2, 128, 48])
    gmix_d = din("gmix", [2, 128, 8])
    gffn_d = din("gffn", [2, 128, 8])
    w_in_d = din("w_in", [2, D, 2560])
    qg_d = din("qg", [2, 128, 1])
    kg_d = din("kg", [2, 128, 1])
    lng_d = din("lng", [2, 128, 512])
    lnb_d = din("lnb", [2, 128, 512])
    wsT_d = din("wsT", [2, 8, 128, 128])
    bsF_d = din("bsF", [2, 128, 4, 512])
    w_out_d = din("w_out", [2, D, D])
    fg_d = din("ffn_g", [D, 2816])
    fu_d = din("ffn_u", [D, 2816])
    fd_d = din("ffn_d", [2816, D])
    rw_d = din("rw", [128, 8, 8])
    rb_d = din("rb", [128, 8])
    mg_d = din("moe_g", [8, D, 3584])
    mu_d = din("moe_u", [8, D, 3584])
    md_d = din("moe_d", [8, 3584, D])
    out_d = nc.dram_tensor("out", [NSEQ, S, D], F32, kind="ExternalOutput").ap()
    dbg_d = None
    if dbg:
        dbg_d = nc.dram_tensor("dbg", [3, S, D], F32, kind="ExternalOutput").ap()

    P = Prog(nc, same_engine_sync=same_engine_sync)

    xT = P.sbuf("xT", [128, 8, S], F32)
    r_x = [[Res(f"x{j}_{c}") for c in range(8)] for j in range(4)]
    hT = P.sbuf("hT", [128, 8, S], BF16)
    r_h = [Res(f"h{j}") for j in range(4)]
    SH = P.sbuf("SH", [128, 8, 4096], BF16)
    r_sh = [Res(f"sh{i}") for i in range(8)]
    ft = P.sbuf("ft", [128, 8, 512], F32)
    r_ft = [Res(f"ft{i}") for i in range(8)]
    bt = P.sbuf("bt", [128, 6, 512], BF16)
    r_bt = [Res(f"bt{i}") for i in range(6)]
    ident = P.sbuf("ident", [128, 128], F32)
    ones_f = P.sbuf("ones_f", [128, 128], F32)
    ones_b = P.sbuf("ones_b", [128, 128], BF16)
    blk_b = P.sbuf("blk_b", [128, 128], BF16)
    negtri = P.sbuf("negtri", [128, 128], BF16)
    neglow = P.sbuf("neglow", [128, 128], BF16)
    maskL = P.sbuf("maskL", [128, 128], F32)
    maskB = P.sbuf("maskB", [128, 128], F32)
    r_const = Res("const")
    cact = P.sbuf("cact", [128, 8, NSEQ], F32)
    adab = P.sbuf("adab", [128, 2, 48], F32)
    adaT = P.sbuf("adaT", [128, 2, 48, NSEQ], F32)
    gmix = P.sbuf("gmixs", [128, 2, 8], F32)
    gffn = P.sbuf("gffns", [128, 2, 8], F32)
    A1 = P.sbuf("A1", [128, 2, 8, NSEQ], F32)
    A2 = P.sbuf("A2", [128, 2, 8, NSEQ], F32)
    r_ada = Res("ada")
    rw = P.sbuf("rws", [128, 8, 8], F32)
    rb = P.sbuf("rbs", [128, 8], F32)
    bsF = P.sbuf("bsFs", [128, 4, 512], F32)
    lng = P.sbuf("lngs", [128, 512], F32)
    lnb = P.sbuf("lnbs", [128, 512], F32)
    wsT = P.sbuf("wsTs", [128, 8, 128], BF16)
    qgs = P.sbuf("qgs", [128, 1], F32)
    kgs = P.sbuf("kgs", [128, 1], F32)
    r_lc = Res("layerconst")
    r_pws_global = [Res("pw0"), Res("pw1")]
    r_ws = Res("wsT")
    sm = P.sbuf("sm", [128, 64], F32)
    r_sm = Res("sm")
    Gt = P.sbuf("Gt", [128, 16, 8], F32)
    r_G = [Res(f"G{j}") for j in range(4)]
    dgt = P.sbuf("dgt", [128, 2, 128], F32)
    r_dg = [Res("dg0"), Res("dg1")]

    pb = [P.psum(f"pb{i}", [128, 512], F32) for i in range(8)]
    r_pb = [Res(f"pb{i}") for i in range(8)]

    r_out = [Res(f"out{i}") for i in range(4)]
    out_ctr = [0]

    def mm(out, lhsT, rhs, start, stop, R, W, sgc=False):
        if sgc:
            P.op("tensor", lambda e: e.matmul(out, lhsT=lhsT, rhs=rhs, start=start, stop=stop, skip_group_check=True), R, W)
        else:
            P.op("tensor", lambda e: e.matmul(out, lhsT=lhsT, rhs=rhs, start=start, stop=stop), R, W)

    def tr(out, in_, R, W):
        P.op("tensor", lambda e: e.transpose(out, in_, ident[:]), list(R) + [r_const], W)

    def act(out, in_, func, R, W, bias=None, scale=None):
        kw = {}
        if bias is not None:
            kw["bias"] = bias
        if scale is not None:
            kw["scale"] = scale
        if func == AF.Copy and not kw:
            P.op("scalar", lambda e: e.copy(out=out, in_=in_), R, W)
        else:
            P.op("scalar", lambda e: e.activation(out=out, in_=in_, func=func, **kw), R, W)

    def vop(eng, name, R, W, **kw):
        P.op(eng, lambda e: getattr(e, name)(**kw), R, W)

    def cols(j):
        return slice(j * 512, (j + 1) * 512)

    dumped = set()

    def dump(name, ap, R):
        if not probe or name in dumped:
            return
        dumped.add(name)
        shape = list(ap.shape)
        d = nc.dram_tensor("p_" + name, shape, F32, kind="ExternalOutput").ap()
        rd = Res("p_" + name)
        P.dma("gpsimd", d, ap, R, [rd], rd)
        r_out.append(rd)

    ftp = RR([(ft[:, i, :], r_ft[i]) for i in range(8)])
    ftp4 = RR([(ft[:, i, :], r_ft[i]) for i in range(4)])
    ftp3 = RR([(ft[:, i, :], r_ft[i]) for i in range(3)])
    btp = RR([(bt[:, i, :], r_bt[i]) for i in range(6)])

    def build_consts():
        g = "gpsimd"
        W = [r_const]
        vop(g, "memset", [], W, ap=ident[:], constant=1.0)
        P.op(g, lambda e: e.affine_select(out=ident[:], in_=ident[:], pattern=[[-1, 128]], compare_op=ALU.is_equal,
                                          fill=0.0, base=0, channel_multiplier=1), W, W)
        vop(g, "memset", [], W, ap=ones_f[:], constant=1.0)
        vop(g, "memset", [], W, ap=ones_b[:], constant=1.0)
        vop(g, "memset", [], W, ap=blk_b[:], constant=0.0)
        vop(g, "memset", W, W, ap=blk_b[0:64, 0:64], constant=1.0 / 64)
        vop(g, "memset", W, W, ap=blk_b[64:128, 64:128], constant=1.0 / 64)
        t0, rt0 = ft[:, 0, 0:128], r_ft[0]
        vop(g, "memset", [], [rt0], ap=t0, constant=-1.0)
        P.op(g, lambda e: e.affine_select(out=t0, in_=t0, pattern=[[-1, 128]], compare_op=ALU.is_gt,
                                          fill=0.0, base=0, channel_multiplier=1), [rt0], [rt0])
        vop(g, "tensor_copy", [rt0], W, out=negtri[:], in_=t0)
        vop(g, "memset", W, [rt0], ap=t0, constant=-1.0)
        P.op(g, lambda e: e.affine_select(out=t0, in_=t0, pattern=[[1, 128]], compare_op=ALU.is_ge,
                                          fill=0.0, base=0, channel_multiplier=-1), [rt0], [rt0])
        vop(g, "tensor_copy", [rt0], W, out=neglow[:], in_=t0)
        vop(g, "memset", [], W, ap=maskL[:], constant=1.0)
        P.op(g, lambda e: e.affine_select(out=maskL[:], in_=maskL[:], pattern=[[1, 128]], compare_op=ALU.is_gt,
                                          fill=0.0, base=0, channel_multiplier=-1), W, W)
        vop(g, "memset", [], W, ap=maskB[:], constant=0.0)
        P.op(g, lambda e: e.affine_select(out=maskB[:], in_=maskB[:], pattern=[[1, 128]], compare_op=ALU.is_gt,
                                          fill=-30000.0, base=0, channel_multiplier=-1), W, W)
        P.dma("sync", cact[:], cT_d, [], [r_ada], r_ada)
        P.dma("sync", adab[:], ada_b_d.rearrange("l p j -> p l j"), [], [r_ada], r_ada)
        P.dma("sync", gmix[:], gmix_d.rearrange("l p j -> p l j"), [], [r_ada], r_ada)
        P.dma("sync", gffn[:], gffn_d.rearrange("l p j -> p l j"), [], [r_ada], r_ada)
        P.dma("sync", rw[:], rw_d, [], [r_const], r_const)
        P.dma("sync", rb[:], rb_d, [], [r_const], r_const)
        act(cact[:], cact[:], AF.Silu, [r_ada], [r_ada])

    def ada_phase():
        stg = [xT[:, 0:2, :].rearrange("p a (k n) -> p (a k) n", n=512),
               xT[:, 2:4, :].rearrange("p a (k n) -> p (a k) n", n=512)]
        r_stg = [Res("adastg0"), Res("adastg1")]
        pa = pb[0]
        for l in range(2):
            for pc in range(12):
                sb = pc % 2
                P.dma("sync", stg[sb], ada_w_d[l, :, pc * 512:(pc + 1) * 512].rearrange("(k p) n -> p k n", p=128),
                      [], [r_stg[sb]], r_stg[sb])
                for cc in range(4):
                    j = pc * 4 + cc
                    for k in range(8):
                        mm(pa[:, j * NSEQ:(j + 1) * NSEQ], stg[sb][:, k, cc * 128:(cc + 1) * 128], cact[:, k, :],
                           k == 0, k == 7, [r_stg[sb], r_ada], [r_pb[0]])
            pav = pa[:, 0:48 * NSEQ].rearrange("p (j b) -> p j b", b=NSEQ)
            for b in range(NSEQ):
                vop("vector", "tensor_tensor", [r_pb[0], r_ada], [r_ada], out=adaT[:, l, :, b], in0=pav[:, :, b],
                    in1=adab[:, l, :], op=ALU.add)
            for b in range(NSEQ):
                vop("vector", "scalar_tensor_tensor", [r_ada], [r_ada], out=A1[:, l, :, b], in0=adaT[:, l, 8:16, b],
                    scalar=1.0, in1=gmix[:, l, :], op0=ALU.add, op1=ALU.mult)
                vop("vector", "scalar_tensor_tensor", [r_ada], [r_ada], out=A2[:, l, :, b], in0=adaT[:, l, 32:40, b],
                    scalar=1.0, in1=gffn[:, l, :], op0=ALU.add, op1=ALU.mult)
        dump('adaT', adaT[:], [r_ada]); dump('A1', A1[:], [r_ada])
        P.fence(r_stg + [r_ada, r_const, r_pb[0]])

    def load_x(s):
        for t in range(16):
            j = t // 4
            xs_i = t % 2
            xs = ft[:, 4 * xs_i:4 * xs_i + 2, :].rearrange("p a n -> p (a n)")
            rxs = [r_ft[4 * xs_i], r_ft[4 * xs_i + 1]]
            P.dma("sync", xs, x_d[s, t * 128:(t + 1) * 128, :], [], rxs, rxs[0])
            bA, bB = (2 * (t % 4)) % 8, (2 * (t % 4) + 1) % 8
            for c in range(8):
                bk = bA if c < 4 else bB
                tr(pb[bk][:, (c % 4) * 128:(c % 4 + 1) * 128], xs[:, c * 128:(c + 1) * 128], rxs, [r_pb[bk]])
            act(xT[:, 0:4, t * 128:(t + 1) * 128], pb[bA][:].rearrange("p (k n) -> p k n", k=4), AF.Copy,
                [r_pb[bA]], r_x[j][0:4])
            vop("vector", "tensor_copy", [r_pb[bB]], r_x[j][4:8], out=xT[:, 4:8, t * 128:(t + 1) * 128],
                in_=pb[bB][:].rearrange("p (k n) -> p k n", k=4))

    def store_x(dst):
        for t in range(16):
            j = t // 4
            xs_i = t % 2
            xs = ft[:, 4 * xs_i:4 * xs_i + 2, :].rearrange("p a n -> p (a n)")
            rxs = [r_ft[4 * xs_i], r_ft[4 * xs_i + 1]]
            bA, bB = (2 * (t % 4)) % 8, (2 * (t % 4) + 1) % 8
            for c in range(8):
                bk = bA if c < 4 else bB
                tr(pb[bk][:, (c % 4) * 128:(c % 4 + 1) * 128], xT[:, c, t * 128:(t + 1) * 128], [r_x[j][c]], [r_pb[bk]])
            act(xs[:, 0:512], pb[bA][:], AF.Copy, [r_pb[bA]], [rxs[0]])
            vop("vector", "tensor_copy", [r_pb[bB]], [rxs[1]], out=xs[:, 512:1024], in_=pb[bB][:])
            ro = r_out[out_ctr[0] % 4]
            out_ctr[0] += 1
            P.dma("sync", dst[t * 128:(t + 1) * 128, :], xs, rxs, [ro], ro)

    def norm_phase(s, l, which):
        A = A1 if which == 1 else A2
        boff = 0 if which == 1 else 24
        router = (l == 1 and which == 2)
        for j in range(4):
            ssum = pb[j % 2]
            r_ss = r_pb[j % 2]
            for c in range(8):
                sq, rsq = btp.get()
                act(sq, xT[:, c, cols(j)], AF.Square, [r_x[j][c]], [rsq])
                mm(ssum[:], ones_b[:], sq, c == 0, c == 7, [rsq, r_const], [r_ss])
            rs, rrs = ft[:, 3, :], r_ft[3]
            act(rs, ssum[:], AF.Ln, [r_ss], [rrs], bias=EPS, scale=1.0 / D)
            act(rs, rs, AF.Exp, [rrs], [rrs], scale=-0.5)
            for c in range(8):
                tmp, rtmp = ftp3.get()
                vop("vector", "scalar_tensor_tensor", [r_x[j][c], rrs, r_ada], [rtmp], out=tmp, in0=xT[:, c, cols(j)],
                    scalar=A[:, l, c, s:s + 1], in1=rs, op0=ALU.mult, op1=ALU.mult)
                bias = adaT[:, l, boff + c, s:s + 1]
                dump('tmp0', tmp, [rtmp])
                act(hT[:, c, cols(j)], tmp, AF.Identity, [rtmp, r_ada], [r_h[j]], bias=bias, scale=1.0)
                if router:
                    hf, rhf = ftp3.get()
                    act(hf, tmp, AF.Identity, [rtmp, r_ada], [rhf], bias=bias, scale=1.0)
                    for sub in range(4):
                        mm(pb[7][:, sub * 8:(sub + 1) * 8], hf[:, sub * 128:(sub + 1) * 128], rw[:, c, :],
                           (c == 0 and sub == 0), c == 7, [rhf, r_const], [r_pb[7]], sgc=True)
            if j == 0 and l == 0 and which == 1:
                dump('h0', hT[:, :, 0:512], [r_h[0]])
            if router:
                for sub in range(4):
                    gating(pb[7][:, sub * 8:(sub + 1) * 8], j * 4 + sub, j)

    def gating(lg_ps, tt, j):
        v = "vector"
        R = [r_sm]
        Lb, m1, mk1, L2, m2, mk2 = sm[:, 0:8], sm[:, 8:9], sm[:, 16:24], sm[:, 24:32], sm[:, 9:10], sm[:, 32:40]
        d, ed, g1, g2, Ga = sm[:, 10:11], sm[:, 11:12], sm[:, 12:13], sm[:, 13:14], sm[:, 40:48]
        vop(v, "tensor_tensor", [r_pb[7], r_const, r_sm], R, out=Lb, in0=lg_ps, in1=rb[:], op=ALU.add)
        vop(v, "tensor_reduce", R, R, out=m1, in_=Lb, axis=AX.X, op=ALU.max)
        vop(v, "tensor_scalar", R, R, out=mk1, in0=Lb, scalar1=m1, scalar2=None, op0=ALU.is_equal)
        vop(v, "scalar_tensor_tensor", R, R, out=L2, in0=mk1, scalar=-1e30, in1=Lb, op0=ALU.mult, op1=ALU.add)
        vop(v, "tensor_reduce", R, R, out=m2, in_=L2, axis=AX.X, op=ALU.max)
        vop(v, "tensor_scalar", R, R, out=mk2, in0=L2, scalar1=m2, scalar2=None, op0=ALU.is_equal)
        vop(v, "tensor_tensor", R, R, out=d, in0=m2, in1=m1, op=ALU.subtract)
        act(ed, d, AF.Exp, R, R)
        vop(v, "tensor_scalar", R, R, out=g1, in0=ed, scalar1=1.0, scalar2=None, op0=ALU.add)
        vop(v, "reciprocal", R, R, out=g1, in_=g1)
        vop(v, "tensor_tensor", R, R, out=g2, in0=ed, in1=g1, op=ALU.mult)
        vop(v, "tensor_scalar", R, R, out=Ga, in0=mk1, scalar1=g1, scalar2=None, op0=ALU.mult)
        vop(v, "scalar_tensor_tensor", R, [r_sm, r_G[j]], out=Gt[:, tt, :], in0=mk2, scalar=g2, in1=Ga,
            op0=ALU.mult, op1=ALU.add)

    def slot_kn(i, w):
        return SH[:, i, 0:8 * w].rearrange("p (k n) -> p k n", k=8)

    def load_cols(i, src2d, c0, w):
        v = slot_kn(i, w)
        P.dma("gpsimd", v, src2d[:, c0:c0 + w].rearrange("(k p) n -> p k n", p=128), [], [r_sh[i]], r_sh[i])
        return v

    def load_rows(i, src2d, r0, nrows):
        nch = nrows // 128
        v = SH[:, i, 0:nch * 1024].rearrange("p (c d) -> p c d", c=nch)
        P.dma("gpsimd", v, src2d[r0:r0 + nrows, :].rearrange("(c p) d -> p c d", p=128), [], [r_sh[i]], r_sh[i])
        return v

    def x_update(s, l, j, dc, ybank, rbank, goff):
        vop("vector", "scalar_tensor_tensor", [rbank, r_ada, r_x[j][dc]], [r_x[j][dc]], out=xT[:, dc, cols(j)],
            in0=ybank[:], scalar=adaT[:, l, goff + dc, s:s + 1], in1=xT[:, dc, cols(j)], op0=ALU.mult, op1=ALU.add)

    def mixer_phase(s, l):
        W = [r_lc]
        P.dma("sync", bsF[:], bsF_d[l], [], W, r_lc)
        P.dma("sync", lng[:], lng_d[l], [], W, r_lc)
        P.dma("sync", lnb[:], lnb_d[l], [], W, r_lc)
        P.dma("sync", qgs[:], qg_d[l], [], W, r_lc)
        P.dma("sync", kgs[:], kg_d[l], [], W, r_lc)
        P.dma("gpsimd", wsT[:], wsT_d[l].rearrange("g j i -> j g i"), [], [r_ws], r_ws)
        vop("gpsimd", "memset", [r_ws], [r_ws], ap=wsT[64:128, :, 0:64], constant=0.0)
        vop("vector", "tensor_scalar", W, W, out=qgs[:], in0=qgs[:], scalar1=0.125, scalar2=None, op0=ALU.mult)

        Wvg = load_cols(1, w_in_d[l], 2048, 512)
        Wu = load_cols(2, w_in_d[l], 1536, 512)
        WoB = load_rows(3, w_out_d[l], 512, 512)
        vn = SH[:, 5, 2048:4096].rearrange("p (b n) -> p b n", b=4)
        r_vn = r_sh[5]
        obs = [SH[:, 7, 0:2048].rearrange("p (c n) -> p c n", c=4), SH[:, 7, 2048:4096].rearrange("p (c n) -> p c n", c=4)]
        r_ob = [Res("ob0"), Res("ob1")]
        r_spb = [Res(f"spb{i}") for i in range(8)]
        qTs = [SH[:, 4, 0:2048], SH[:, 2, 0:2048]]
        kTs = [SH[:, 4, 2048:4096], SH[:, 2, 2048:4096]]
        vts = [SH[:, 5, 0:2048].rearrange("p (t n) -> p t n", t=16), SH[:, 5, 2048:4096].rearrange("p (t n) -> p t n", t=16)]
        pws = [SH[:, 0, 0:3072].rearrange("p (i k n) -> p i k n", i=3, k=8), SH[:, 3, 0:3072].rearrange("p (i k n) -> p i k n", i=3, k=8)]
        WoPs = [SH[:, 0, 3072:4096], SH[:, 3, 3072:4096]]
        r_qs, r_ks, r_vts = ([Res(f"{n}{i}") for i in range(2)] for n in ("q", "k", "vt"))
        r_pws = r_pws_global
        oas = [SH[:, 6, 0:2048], SH[:, 6, 2048:4096]]
        r_oa = [[Res(f"oa{i}_{g}") for g in range(4)] for i in range(2)]
        PB = 6

        def proj_gen(cc):
            b = cc % 2
            pw, r_pw = pws[b], r_pws[b]
            qf, rqf = ft[:, 6, :], r_ft[6]
            rs, rrs = ft[:, 7, :], r_ft[7]
            sq, rsq = bt[:, 5, :], r_bt[5]
            for i, c0 in enumerate((cc * 128, 512 + cc * 128, 1024 + cc * 128)):
                P.dma("gpsimd", pw[:, i, :, :], w_in_d[l][:, c0:c0 + 128].rearrange("(k p) n -> p k n", p=128),
                      [], [r_pw], r_pw)
            P.dma("gpsimd", WoPs[b], w_out_d[l][cc * 128:(cc + 1) * 128, :], [], [r_pw], r_pw)
            yield
            for j in range(4):
                for i, (dst, rdst, gvec) in enumerate(((qTs[b], r_qs[b], qgs), (kTs[b], r_ks[b], kgs))):
                    for k in range(8):
                        mm(pb[PB][:], pw[:, i, k, :], hT[:, k, cols(j)], k == 0, k == 7, [r_h[j], r_pw], [r_pb[PB]])
                        if k % 2 == 1:
                            yield
                    act(qf, pb[PB][:], AF.Copy, [r_pb[PB]], [rqf])
                    act(sq, pb[PB][:], AF.Square, [r_pb[PB]], [rsq])
                    yield
                    mm(pb[PB][:], blk_b[:], sq, True, True, [rsq, r_const], [r_pb[PB]])
                    yield
                    act(rs, pb[PB][:], AF.Ln, [r_pb[PB]], [rrs], bias=EPS, scale=1.0)
                    act(rs, rs, AF.Exp, [rrs], [rrs], scale=-0.5)
                    yield
                    vop("vector", "scalar_tensor_tensor", [rqf, rrs, r_lc], [rdst], out=dst[:, cols(j)], in0=qf,
                        scalar=gvec[:, 0:1], in1=rs, op0=ALU.mult, op1=ALU.mult)
                    yield
                for blk in range(4):
                    tcols = slice(j * 512 + blk * 128, j * 512 + (blk + 1) * 128)
                    for k in range(8):
                        mm(pb[PB][:, blk * 128:(blk + 1) * 128], hT[:, k, tcols], pw[:, 2, k, :], k == 0, k == 7,
                           [r_h[j], r_pw], [r_pb[PB]])
                        if k % 4 == 3:
                            yield
                act(vts[b][:, j * 4:(j + 1) * 4, :], pb[PB][:].rearrange("p (t n) -> p t n", t=4), AF.Copy,
                    [r_pb[PB]], [r_vts[b]])
                yield

        r_gst = [Res(f"gst{i}") for i in range(4)]
        mvv = sm[:, 24:32].rearrange("p (b t) -> p b t", t=2)
        ftp_hi = RR([(ft[:, 4 + i, :], r_ft[4 + i]) for i in range(2)])
        def gate_gen():
            for j in (range(4) if 'gate' not in skip else ()):
                for blk in range(4):
                    tcols = slice(j * 512 + blk * 128, j * 512 + (blk + 1) * 128)
                    bk = blk
                    for k in range(8):
                        mm(pb[bk][:], hT[:, k, tcols], Wvg[:, k, :], k == 0, k == 7, [r_h[j], r_sh[1]], [r_pb[bk]])
                    gv, rgv = ft[:, blk, :], r_ft[blk]
                    act(gv, pb[bk][:], AF.Gelu, [r_pb[bk]], [rgv])
                    vop("vector", "bn_stats", [rgv], [r_gst[blk]], out=sm[:, 6 * blk:6 * blk + 6], in_=gv)
                    vop("vector", "bn_aggr", [r_gst[blk], r_sm], [r_gst[blk]], out=mvv[:, blk, :], in_=sm[:, 6 * blk:6 * blk + 6])
                    yield
                act(sm[:, 32:36], mvv[:, :, 1], AF.Sqrt, r_gst + [r_sm], [r_sm], bias=EPS, scale=1.0)
                vop("vector", "reciprocal", [r_sm], [r_sm], out=sm[:, 32:36], in_=sm[:, 32:36])
                for blk in range(4):
                    gv, rgv = ft[:, blk, :], r_ft[blk]
                    vop("vector", "tensor_scalar", [rgv, r_sm, r_gst[blk]], [rgv], out=gv, in0=gv, scalar1=mvv[:, blk, 0:1],
                        scalar2=sm[:, 32 + blk:33 + blk], op0=ALU.subtract, op1=ALU.mult)
                    vop("vector", "tensor_tensor", [rgv, r_lc], [rgv], out=gv, in0=gv, in1=lng[:], op=ALU.mult)
                    vop("gpsimd", "tensor_tensor", [rgv, r_lc], [r_vn], out=vn[:, blk, :], in0=gv, in1=lnb[:], op=ALU.add)
                    yield
                ob = obs[j % 2]
                rob = r_ob[j % 2]
                dump('vn0', vn, [r_vn])
                for cc in range(4):
                    for blk in range(4):
                        for gi in range(2):
                            bk = 2 + gi
                            mm(pb[bk][:, blk * 128:(blk + 1) * 128], vn[:, blk, cc * 128:(cc + 1) * 128],
                               wsT[:, 2 * cc + gi, :], True, True, [r_vn, r_ws], [r_pb[bk]])
                    mT, rmT = ftp_hi.get()
                    vop("vector", "tensor_tensor", [r_pb[2], r_lc], [rmT], out=mT[0:64, :], in0=pb[2][0:64, :],
                        in1=bsF[0:64, cc, :], op=ALU.add)
                    vop("vector", "tensor_tensor", [r_pb[3], r_lc], [rmT], out=mT[64:128, :], in0=pb[3][64:128, :],
                        in1=bsF[64:128, cc, :], op=ALU.add)
                    yield
                    bk = 4 + cc % 2
                    for k in range(8):
                        mm(pb[bk][:], Wu[:, k, cc * 128:(cc + 1) * 128], hT[:, k, cols(j)], k == 0, k == 7,
                           [r_h[j], r_sh[2]], [r_pb[bk]])
                    gu, rgu = ftp_hi.get()
                    act(gu, pb[bk][:], AF.Gelu, [r_pb[bk]], [rgu])
                    vop("gpsimd", "tensor_tensor", [rgu, rmT], [rob], out=ob[:, cc, :], in0=gu, in1=mT, op=ALU.mult)
                    yield
                dump('ob0', ob, [rob])
                for dc in range(8):
                    bk = (7, 0)[dc % 2]
                    for k4 in range(4):
                        mm(pb[bk][:], WoB[:, k4, dc * 128:(dc + 1) * 128], ob[:, k4, :], k4 == 0, k4 == 3,
                           [rob, r_sh[3]], [r_pb[bk]])
                    x_update(s, l, j, dc, pb[bk], r_pb[bk], 16)
                    if dc % 2 == 1:
                        yield


        pairs = list(range(4)) if 'attn' not in skip else []
        gens = []
        if 'gate' not in skip:
            gens.append(gate_gen())
        if pairs:
            gens.append(proj_gen(0))
        while gens:
            for g_ in list(gens):
                try:
                    next(g_)
                except StopIteration:
                    gens.remove(g_)
        P.fence([r_sh[1], r_sh[2], r_sh[3], r_sh[5], r_sh[7]] + r_ob)
        for cc in pairs:
            b = cc % 2
            qT, kT, vt, WoP = qTs[b], kTs[b], vts[b], WoPs[b]
            r_q, r_k, r_vt, r_pw = r_qs[b], r_ks[b], r_vts[b], r_pws[b]
            nxt = proj_gen(cc + 1) if cc + 1 < 4 else iter(())
            oa = oas[cc % 2]
            roa = r_oa[cc % 2]
            steps = []
            for g in range(4):
                for kb in range(4 * g + 3, -1, -1):
                    for h in range(2):
                        col0 = max(0, kb * 128 - g * 512)
                        steps.append(dict(g=g, kb=kb, h=h, po=64 * h, first=(kb == 4 * g + 3), last=(kb == 0),
                                          diag=(kb >= 4 * g), col0=col0, wd=512 - col0))
            e_pool = RR([(ft[:, i, :], r_ft[i]) for i in range(6)])
            spb_pool = RR([(SH[:, 1, i * 512:(i + 1) * 512], r_spb[i]) for i in range(8)])
            wt_pool = RR([(bt[:, i, :], r_bt[i]) for i in range(5)])
            sbanks = [0, 1, 7]

            def stS(st, t):
                po, wd, kb, g, col0 = st["po"], st["wd"], st["kb"], st["g"], st["col0"]
                bk = sbanks[t % 3]
                st["S"], st["rS"] = pb[bk], r_pb[bk]
                mm(pb[bk][:, 0:wd], kT[po:po + 64, kb * 128:(kb + 1) * 128],
                   qT[po:po + 64, g * 512 + col0:(g + 1) * 512], True, True, [r_q, r_k], [r_pb[bk]])

            def stA1(st):
                wd = st["wd"]
                e_, re_ = e_pool.get()
                spb, rspb = spb_pool.get()
                st["e"], st["re"], st["spb"], st["rspb"] = e_, re_, spb, rspb
                act(e_[:, 0:wd], st["S"][:, 0:wd], AF.Exp, [st["rS"]], [re_])
                act(spb[:, 0:wd], e_[:, 0:wd], AF.Ln, [re_], [rspb], bias=1.0, scale=1.0)
                if st["diag"]:
                    vop("gpsimd", "tensor_tensor", [rspb, r_const], [rspb], out=spb[:, 0:128], in0=spb[:, 0:128],
                        in1=maskL[:], op=ALU.mult)

            def stA2(st):
                wd = st["wd"]
                vop("vector", "tensor_tensor", [st["rS"], st["rspb"], st["re"]], [st["re"]], out=st["e"][:, 0:wd],
                    in0=st["S"][:, 0:wd], in1=st["spb"][:, 0:wd], op=ALU.subtract)

            def stB1(st):
                h, wd, col0 = st["h"], st["wd"], st["col0"]
                mm(pb[2 + h][:, col0:512], negtri[:], st["spb"][:, 0:wd], st["first"], True,
                   [st["rspb"], r_const], [r_pb[2 + h]], sgc=True)

            def stB2(st):
                h, wd, col0 = st["h"], st["wd"], st["col0"]
                e_, re_ = st["e"], st["re"]
                vop("vector", "tensor_tensor", [r_pb[2 + h], re_], [re_], out=e_[:, 0:wd], in0=pb[2 + h][:, col0:512],
                    in1=e_[:, 0:wd], op=ALU.add)
                if st["diag"]:
                    vop("vector", "tensor_tensor", [re_, r_const], [re_], out=e_[:, 0:128], in0=e_[:, 0:128],
                        in1=maskB[:], op=ALU.add)

            def stB3(st):
                wd = st["wd"]
                wt, rwt = wt_pool.get()
                st["wt"], st["rwt"] = wt, rwt
                act(wt[:, 0:wd], st["e"][:, 0:wd], AF.Exp, [st["re"]], [rwt])

            def stC1(st):
                h, wd, col0 = st["h"], st["wd"], st["col0"]
                mm(pb[2 + h][:, col0:512], neglow[:], st["spb"][:, 0:wd], False, True,
                   [st["rspb"], r_const], [r_pb[2 + h]], sgc=True)

            def stC2(st):
                h, wd, col0, kb, g, po = st["h"], st["wd"], st["col0"], st["kb"], st["g"], st["po"]
                Ob, rO = pb[4 + h], r_pb[4 + h]
                mm(Ob[:, col0:512], vt[:, kb, :], st["wt"][:, 0:wd], st["first"], st["last"], [r_vt, st["rwt"]], [rO],
                   sgc=True)
                if st["last"]:
                    act(oa[po:po + 64, cols(g)], Ob[po:po + 64, :], AF.Copy, [rO], [roa[g]])
                    if h == 1:
                        j = g
                        for dc in range(8):
                            wb = 4 + dc % 2
                            mm(pb[wb][:], WoP[:, dc * 128:(dc + 1) * 128], oa[:, cols(j)], True, True,
                               [roa[j], r_pw], [r_pb[wb]])
                            x_update(s, l, j, dc, pb[wb], r_pb[wb], 16)

            T = len(steps)

            def at(i):
                return steps[i] if 0 <= i < T else None

            for t in range(-1, T + 6):
                for fn, i in ((stC1, t - 4), (stB1, t - 2), (stC2, t - 5)):
                    if at(i) is not None:
                        fn(at(i))
                if at(t + 1) is not None:
                    stS(at(t + 1), t + 1)
                for fn, i in ((stB3, t - 4), (stA1, t), (stB2, t - 3), (stA2, t - 1)):
                    if at(i) is not None:
                        fn(at(i))
                if t >= 2:
                    next(nxt, None)
                    if t % 3 == 0:
                        next(nxt, None)
            for _ in nxt:
                pass
        P.fence(r_sh + r_ob + r_spb + r_qs + r_ks + r_vts + r_pws + r_oa[0] + r_oa[1] + r_h + r_pb + r_ft + r_bt)

    def ffn_phase(s, l):
        if l == 0:
            groups = [(None, gd, fg_d, fu_d, fd_d, f0, fw) for gd, (f0, fw) in
                      enumerate([(0, 512), (512, 512), (1024, 512), (1536, 512), (2048, 512), (2560, 256)])]
        else:
            groups = [(e, fgi, mg_d[e], mu_d[e], md_d[e], fgi * 512, 512) for e in range(8) for fgi in range(7)]
        hids = [SH[:, 6, 0:2048].rearrange("p (c n) -> p c n", c=4), SH[:, 6, 2048:4096].rearrange("p (c n) -> p c n", c=4)]
        r_hid = [[Res(f"hid{b}_{c}") for c in range(4)] for b in range(2)]
        gbc = [ft[:, 4 + j, :] for j in range(4)]
        r_gbc = [r_ft[4 + j] for j in range(4)]
        slot_ctr = [0]

        def issue_loads(grp):
            e, gi, gsrc, usrc, dsrc, f0, fw = grp
            base = (slot_ctr[0] % 2) * 3
            slot_ctr[0] += 1
            Wg = load_cols(base, gsrc, f0, fw)
            Wu = load_cols(base + 1, usrc, f0, fw)
            Wd = load_rows(base + 2, dsrc, f0, fw)
            return (Wg, Wu, Wd, base)

        pend = issue_loads(groups[0])
        hctr = 0
        yctr = 0
        for gidx, grp in enumerate(groups):
            e, gi, gsrc, usrc, dsrc, f0, fw = grp
            Wg, Wu, Wd, base = pend
            if gidx + 1 < len(groups):
                pend = issue_loads(groups[gidx + 1])
            nfc = fw // 128
            if e is not None and gi == 0:
                for tt in range(16):
                    j = tt // 4
                    dg, rdg = dgt[:, tt % 2, :], r_dg[tt % 2]
                    vop("vector", "tensor_scalar", [r_G[j], r_const], [rdg], out=dg, in0=ident[:],
                        scalar1=Gt[:, tt, e:e + 1], scalar2=None, op0=ALU.mult)
                    bk = 6 + j % 2
                    mm(pb[bk][:, (tt % 4) * 128:(tt % 4 + 1) * 128], ones_f[:], dg, True, True, [rdg, r_const], [r_pb[bk]])
                    if tt % 4 == 3:
                        act(gbc[j], pb[bk][:], AF.Copy, [r_pb[bk]], [r_gbc[j]])
            for j in range(4):
                hid = hids[hctr % 2]
                rhid = r_hid[hctr % 2]
                hctr += 1
                for fc in range(nfc):
                    bg, bu = fc % 2, 2 + fc % 2
                    for k in range(8):
                        mm(pb[bg][:], Wg[:, k, fc * 128:(fc + 1) * 128], hT[:, k, cols(j)], k == 0, k == 7,
                           [r_h[j], r_sh[base]], [r_pb[bg]])
                    for k in range(8):
                        mm(pb[bu][:], Wu[:, k, fc * 128:(fc + 1) * 128], hT[:, k, cols(j)], k == 0, k == 7,
                           [r_h[j], r_sh[base + 1]], [r_pb[bu]])
                    sg, rsg = ftp4.get()
                    act(sg, pb[bg][:], AF.Silu, [r_pb[bg]], [rsg])
                    vop("vector", "tensor_tensor", [rsg, r_pb[bu]], [rhid[fc]], out=hid[:, fc, :], in0=pb[bu][:], in1=sg,
                        op=ALU.mult)
                for dc in range(8):
                    bk = 4 + yctr % 4 if e is None else 4 + yctr % 2
                    yctr += 1
                    for fc in range(nfc):
                        mm(pb[bk][:], Wd[:, fc, dc * 128:(dc + 1) * 128], hid[:, fc, :], fc == 0, fc == nfc - 1,
                           [rhid[fc], r_sh[base + 2]], [r_pb[bk]])
                    if e is None:
                        x_update(s, l, j, dc, pb[bk], r_pb[bk], 40)
                    else:
                        tmp, rtmp = ftp4.get()
                        vop("vector", "scalar_tensor_tensor", [r_pb[bk], r_ada, r_gbc[j]], [rtmp], out=tmp, in0=pb[bk][:],
                            scalar=adaT[:, l, 40 + dc, s:s + 1], in1=gbc[j], op0=ALU.mult, op1=ALU.mult)
                        vop("gpsimd", "tensor_tensor", [rtmp, r_x[j][dc]], [r_x[j][dc]], out=xT[:, dc, cols(j)],
                            in0=xT[:, dc, cols(j)], in1=tmp, op=ALU.add)
        P.fence(r_sh + r_hid[0] + r_hid[1] + r_h + r_pb + r_ft)

    build_consts()
    ada_phase()
    stage = [0]

    def dbg_dump(s):
        if dbg and s == 0 and stage[0] < 3:
            store_x(dbg_d[stage[0]])
            P.fence(r_ft + r_pb)
        stage[0] += 1

    for s in range(NSEQ):
        stage[0] = 0
        load_x(s)
        P.fence(r_ft + r_pb)
        done = False
        for l in range(2):
            norm_phase(s, l, 1)
            mixer_phase(s, l)
            dbg_dump(s)
            if stop_after == (l, "mix"):
                done = True
                break
            norm_phase(s, l, 2)
            ffn_phase(s, l)
            if l == 0:
                dbg_dump(s)
            if stop_after == (l, "ffn"):
                done = True
                break
        store_x(out_d[s])
        P.fence(r_ft + r_pb + [r_x[j][c] for j in range(4) for c in range(8)])
    P.op("sync", None, r_out, [])
    P.emit()
    P.close()
    return nc, len(P.ops)


def make_in_map(inp, b0, nseq):
    f = lambda a: np.ascontiguousarray(np.asarray(a, dtype=np.float32))
    fm = lambda v, n: f(np.asarray(v).reshape(2, n, 128).transpose(0, 2, 1))
    c = np.asarray(inp["c"])[b0:b0 + nseq]
    bs = np.asarray(inp["sg_b_spatial"])
    bsF = np.repeat(bs, 64, axis=1).reshape(2, 4, 128, 128)
    bsF = np.tile(bsF, (1, 1, 1, 4)).transpose(0, 2, 1, 3)
    m = {
        "x": f(np.asarray(inp["x"])[b0:b0 + nseq]),
        "cT": f(c.T.reshape(8, 128, nseq).transpose(1, 0, 2)),
        "ada_w": f(inp["ada_w"]),
        "ada_b": fm(inp["ada_b"], 48),
        "gmix": fm(inp["norm_mix_g"], 8),
        "gffn": fm(inp["norm_ffn_g"], 8),
        "w_in": f(inp["w_in"]),
        "qg": f(np.tile(np.asarray(inp["q_norm_g"]), (1, 2)).reshape(2, 128, 1)),
        "kg": f(np.tile(np.asarray(inp["k_norm_g"]), (1, 2)).reshape(2, 128, 1)),
        "lng": f(np.broadcast_to(np.asarray(inp["sg_ln_g"])[:, None, :], (2, 128, 512))),
        "lnb": f(np.broadcast_to(np.asarray(inp["sg_ln_b"])[:, None, :], (2, 128, 512))),
        "wsT": f(np.asarray(inp["sg_w_spatial"]).transpose(0, 1, 3, 2)),
        "bsF": f(bsF),
        "w_out": f(inp["w_out"]),
        "ffn_g": f(np.asarray(inp["ffn_w_gate"])[0]),
        "ffn_u": f(np.asarray(inp["ffn_w_up"])[0]),
        "ffn_d": f(np.asarray(inp["ffn_w_down"])[0]),
        "rw": f(np.asarray(inp["router_w"])[0].reshape(8, 128, 8).transpose(1, 0, 2)),
        "rb": f(np.broadcast_to(np.asarray(inp["router_b"])[0][None, :], (128, 8))),
        "moe_g": f(np.asarray(inp["moe_w_gate"])[0]),
        "moe_u": f(np.asarray(inp["moe_w_up"])[0]),
        "moe_d": f(np.asarray(inp["moe_w_down"])[0]),
    }
    return m


_NC_CACHE = {}


def kernel(**inputs):
    nseq = 4
    if "nc" not in _NC_CACHE:
        _NC_CACHE["nc"] = build(NSEQ=nseq)[0]
    nc = _NC_CACHE["nc"]
    shared = make_in_map(inputs, 0, nseq)
    in_maps = []
    for core in range(NCORES):
        m = dict(shared)
        c = np.asarray(inputs["c"])[core * nseq:(core + 1) * nseq]
        m["x"] = np.ascontiguousarray(np.asarray(inputs["x"], dtype=np.float32)[core * nseq:(core + 1) * nseq])
        m["cT"] = np.ascontiguousarray(c.T.reshape(8, 128, nseq).transpose(1, 0, 2).astype(np.float32))
        in_maps.append(m)
    res = run_bass_kernel_spmd(nc, in_maps, core_ids=list(range(NCORES)))
    out = np.concatenate([np.asarray(r["out"]) for r in res.results], axis=0)
    return out.astype(np.float32)
```
